# Optimizing a Trainium2 kernel written in Bass

```python
import math
import jax, jax.numpy as jnp
from jax import lax
import numpy as np


D_MODEL = 2048
BATCH = 2
SEQ = 16384
DEPTH = 2

GRID_W = 64
CTX_LEN = 256
ATTN_WIDTH = D_MODEL // 2
POOL_WIDTH = D_MODEL - ATTN_WIDTH
N_HEADS = 8
HEAD_DIM = ATTN_WIDTH // (2 * N_HEADS)
V_DIM = 2 * HEAD_DIM
POOL_WINDOWS = (2, 4, 8, 16)
N_POOL_GROUPS = len(POOL_WINDOWS)
POOL_GROUP = POOL_WIDTH // N_POOL_GROUPS
IN_WIDTH = 3 * ATTN_WIDTH + POOL_WIDTH
N_EXPERTS = 16
N_EXPERT_GROUPS = 4
EXPERTS_PER_GROUP = N_EXPERTS // N_EXPERT_GROUPS
TOP_K = 2
D_EXPERT = 1408
ROPE_THETA = 10000.0
Q_BLOCK = 128
MOE_BLOCK = 128
EPS = 1e-6

kernel_name = 'hybrid_pool_diffattn_grouped_moe_dit'


def rms_norm(x, g):
    xf = x.astype(jnp.float32)
    y = xf * lax.rsqrt(jnp.mean(xf * xf, axis=-1, keepdims=True) + EPS)
    return (y * g.astype(jnp.float32)).astype(x.dtype)


def modulate(h, shift, scale):
    return h * (1.0 + scale) + shift


def axial_rope_tables(n):
    rows = n // GRID_W
    row = jnp.repeat(jnp.arange(rows), GRID_W).astype(jnp.float32)
    col = jnp.tile(jnp.arange(GRID_W), rows).astype(jnp.float32)
    nf = HEAD_DIM // 4
    inv = ROPE_THETA ** (-jnp.arange(nf, dtype=jnp.float32) / nf)
    ang = jnp.stack([row[:, None] * inv, col[:, None] * inv], axis=1)
    return jnp.cos(ang), jnp.sin(ang)


def apply_axial_rope(x, cos, sin):
    B, n, H, M, Dh = x.shape
    nf = Dh // 4
    xr = x.astype(jnp.float32).reshape(B, n, H, M, 2, 2, nf)
    x1, x2 = xr[..., 0, :], xr[..., 1, :]
    cs = cos[None, :, None, None]
    sn = sin[None, :, None, None]
    out = jnp.stack([x1 * cs - x2 * sn, x2 * cs + x1 * sn], axis=-2)
    return out.reshape(B, n, H, M, Dh).astype(x.dtype)


def diff_softmax_attend(q, k, v, lam):
    s = jnp.einsum('bqhmd,bkhmd->bhmqk', q, k, preferred_element_type=jnp.float32)
    p = jax.nn.softmax(s, axis=-1)
    a = p[:, :, 0] - lam * p[:, :, 1]
    return jnp.einsum('bhqk,bkhe->bqhe', a.astype(v.dtype), v)


def diff_attention_blocks(q, k, v, lam):
    B, n = q.shape[:2]
    nb = n // Q_BLOCK
    qb = q.reshape(B, nb, Q_BLOCK, N_HEADS, 2, HEAD_DIM).transpose(1, 0, 2, 3, 4, 5)
    o = lax.map(lambda qblk: diff_softmax_attend(qblk, k, v, lam), qb)
    return o.transpose(1, 0, 2, 3, 4).reshape(B, n, N_HEADS, V_DIM)


def multi_scale_pool(u, w_pool, pool_scale):
    B, n, _ = u.shape
    ug = u.astype(jnp.float32).reshape(B, n, N_POOL_GROUPS, POOL_GROUP)
    S = jnp.concatenate([jnp.zeros((B, 1, N_POOL_GROUPS, POOL_GROUP), jnp.float32),
                         jnp.cumsum(ug, axis=1)], axis=1)
    t = jnp.arange(n)[:, None]
    half = jnp.array([w // 2 for w in POOL_WINDOWS])[None, :]
    wins = jnp.array(POOL_WINDOWS)[None, :]
    lo = jnp.clip(t - half, 0, n - 1)
    hi = jnp.clip(t - half + wins - 1, 0, n - 1)
    gidx = jnp.arange(N_POOL_GROUPS)[None, :]
    cnt = (hi - lo + 1).astype(jnp.float32)[None, :, :, None]
    mean = (S[:, hi + 1, gidx] - S[:, lo, gidx]) / cnt
    d = (mean - ug).astype(u.dtype)
    y = jnp.einsum('bngc,gcd->bngd', d, w_pool)
    return y.reshape(B, n, POOL_WIDTH) * pool_scale


def route(h, w_router, b_router):
    T = h.shape[0]
    logits = jnp.matmul(h, w_router, preferred_element_type=jnp.float32) + b_router.astype(jnp.float32)
    probs = jax.nn.softmax(logits, axis=-1)
    pg = probs.reshape(T, N_EXPERT_GROUPS, EXPERTS_PER_GROUP)
    group_score = jnp.sum(lax.top_k(pg, 2)[0], axis=-1)
    g = jnp.argmax(group_score, axis=-1)
    in_group = pg[jnp.arange(T), g]
    topv, topi = lax.top_k(in_group, TOP_K)
    experts = g[:, None] * EXPERTS_PER_GROUP + topi
    gates = topv / jnp.sum(topv, axis=-1, keepdims=True)
    return experts, gates


def moe(h, experts, gates, w_gate, w_up, w_down):
    T, D = h.shape
    A = T * TOP_K
    e_flat = experts.reshape(A)
    tok = jnp.repeat(jnp.arange(T), TOP_K)
    g_flat = gates.reshape(A).astype(h.dtype)
    order = jnp.argsort(e_flat)
    e_sorted = e_flat[order]
    counts = jnp.bincount(e_flat, length=N_EXPERTS)
    starts = jnp.cumsum(counts) - counts
    padded = (counts + MOE_BLOCK - 1) // MOE_BLOCK * MOE_BLOCK
    pad_ends = jnp.cumsum(padded)
    pad_starts = pad_ends - padded
    dest = pad_starts[e_sorted] + jnp.arange(A) - starts[e_sorted]
    n_blocks = -(-A // MOE_BLOCK) + N_EXPERTS
    P = n_blocks * MOE_BLOCK
    buf_tok = jnp.full((P,), T, jnp.int32).at[dest].set(tok[order])
    buf_gate = jnp.zeros((P,), h.dtype).at[dest].set(g_flat[order])
    block_e = jnp.minimum(jnp.searchsorted(pad_ends, jnp.arange(n_blocks) * MOE_BLOCK, side='right'),
                          N_EXPERTS - 1)
    h_pad = jnp.concatenate([h, jnp.zeros((1, D), h.dtype)], axis=0)
    xb = h_pad[buf_tok].reshape(n_blocks, MOE_BLOCK, D)

    def expert_block(args):
        xblk, e = args
        a = xblk @ w_gate[e]
        b = xblk @ w_up[e]
        return (jax.nn.silu(a) * b) @ w_down[e]

    yb = lax.map(expert_block, (xb, block_e))
    y = yb.reshape(P, D) * buf_gate[:, None]
    return jnp.zeros((T + 1, D), h.dtype).at[buf_tok].add(y)[:T]


def setup_inputs(seed: int = 0) -> dict:
    key = jax.random.key(seed)
    ks = jax.random.split(key, 24)
    f32 = jnp.float32
    D = D_MODEL

    def nrm(k, shape, s):
        return jax.random.normal(k, shape, f32) * s

    return {
        'x': nrm(ks[0], (BATCH, SEQ, D), 1.0),
        'c': nrm(ks[1], (BATCH, D), 1.0),
        'ctx': nrm(ks[2], (BATCH, CTX_LEN, D), 1.0),
        'c_ctx': nrm(ks[3], (D,), 1.0),
        'w_mod': nrm(ks[4], (DEPTH, D, 6 * D), 0.5 * D ** -0.5),
        'b_mod': nrm(ks[5], (DEPTH, 6 * D), 0.02),
        'norm1': 1.0 + nrm(ks[6], (DEPTH, D), 0.02),
        'norm2': 1.0 + nrm(ks[7], (DEPTH, D), 0.02),
        'w_in': nrm(ks[8], (DEPTH, D, IN_WIDTH), D ** -0.5),
        'q_norm': 1.0 + nrm(ks[9], (DEPTH, HEAD_DIM), 0.02),
        'k_norm': 1.0 + nrm(ks[10], (DEPTH, HEAD_DIM), 0.02),
        'lam_qk': nrm(ks[11], (DEPTH, 4, HEAD_DIM), 0.1),
        'sub_norm': 1.0 + nrm(ks[12], (DEPTH, V_DIM), 0.02),
        'w_pool': nrm(ks[13], (DEPTH, N_POOL_GROUPS, POOL_GROUP, POOL_GROUP), POOL_GROUP ** -0.5),
        'pool_scale': 1.0 + nrm(ks[14], (DEPTH, POOL_WIDTH), 0.02),
        'w_out': nrm(ks[15], (DEPTH, D, D), D ** -0.5),
        'w_router': nrm(ks[16], (D, N_EXPERTS), D ** -0.5),
        'b_router': nrm(ks[17], (N_EXPERTS,), 0.01),
        'w_gate': nrm(ks[18], (DEPTH, N_EXPERTS, D, D_EXPERT), D ** -0.5),
        'w_up': nrm(ks[19], (DEPTH, N_EXPERTS, D, D_EXPERT), D ** -0.5),
        'w_down': nrm(ks[20], (DEPTH, N_EXPERTS, D_EXPERT, D), D_EXPERT ** -0.5),
    }


def reference(x, c, ctx, c_ctx, w_mod, b_mod, norm1, norm2, w_in, q_norm, k_norm, lam_qk, sub_norm,
              w_pool, pool_scale, w_out, w_router, b_router, w_gate, w_up, w_down):
    B, n, D = x.shape
    CL = ctx.shape[1]
    AW = ATTN_WIDTH
    cos, sin = axial_rope_tables(n)
    q_scale = HEAD_DIM ** -0.5
    xc = ctx
    for li in range(DEPTH):
        last = li == DEPTH - 1
        mod = jax.nn.silu(c) @ w_mod[li] + b_mod[li]
        modc = jax.nn.silu(c_ctx) @ w_mod[li] + b_mod[li]
        sh1, sc1, g1, sh2, sc2, g2 = jnp.split(mod[:, None, :], 6, axis=-1)
        sh1c, sc1c, g1c, sh2c, sc2c, g2c = jnp.split(modc, 6)

        lam_init = 0.8 - 0.6 * math.exp(-0.3 * li)
        lv = lam_qk[li].astype(jnp.float32)
        lam = jnp.exp(jnp.sum(lv[0] * lv[1])) - jnp.exp(jnp.sum(lv[2] * lv[3])) + lam_init

        h = modulate(rms_norm(x, norm1[li]), sh1, sc1)
        p = h @ w_in[li]
        q = p[..., :AW].reshape(B, n, N_HEADS, 2, HEAD_DIM)
        k = p[..., AW:2 * AW].reshape(B, n, N_HEADS, 2, HEAD_DIM)
        v = p[..., 2 * AW:3 * AW].reshape(B, n, N_HEADS, V_DIM)
        u = p[..., 3 * AW:]
        q = apply_axial_rope(rms_norm(q, q_norm[li]), cos, sin) * q_scale
        k = apply_axial_rope(rms_norm(k, k_norm[li]), cos, sin)

        hc = modulate(rms_norm(xc, norm1[li]), sh1c, sc1c)
        kvc = hc @ w_in[li][:, AW:3 * AW]
        kc = rms_norm(kvc[..., :AW].reshape(B, CL, N_HEADS, 2, HEAD_DIM), k_norm[li])
        vc = kvc[..., AW:].reshape(B, CL, N_HEADS, V_DIM)

        k_all = jnp.concatenate([k, kc], axis=1)
        v_all = jnp.concatenate([v, vc], axis=1)
        o = diff_attention_blocks(q, k_all, v_all, lam)
        o = rms_norm(o, sub_norm[li]) * (1.0 - lam_init)
        mix = jnp.concatenate([o.reshape(B, n, AW), multi_scale_pool(u, w_pool[li], pool_scale[li])],
                              axis=-1) @ w_out[li]
        x_new = x + g1 * mix

        if not last:
            qc = hc @ w_in[li][:, :AW]
            uc = hc @ w_in[li][:, 3 * AW:]
            qc = rms_norm(qc.reshape(B, CL, N_HEADS, 2, HEAD_DIM), q_norm[li]) * q_scale
            oc = diff_softmax_attend(qc, kc, vc, lam)
            oc = rms_norm(oc, sub_norm[li]) * (1.0 - lam_init)
            mixc = jnp.concatenate([oc.reshape(B, CL, AW), multi_scale_pool(uc, w_pool[li], pool_scale[li])],
                                   axis=-1) @ w_out[li]
            xc = xc + g1c * mixc
        x = x_new

        h2 = modulate(rms_norm(x, norm2[li]), sh2, sc2).reshape(B * n, D)
        if not last:
            h2c = modulate(rms_norm(xc, norm2[li]), sh2c, sc2c).reshape(B * CL, D)
            tokens = jnp.concatenate([h2, h2c], axis=0)
        else:
            tokens = h2
        experts, gates = route(tokens, w_router, b_router)
        y = moe(tokens, experts, gates, w_gate[li], w_up[li], w_down[li])
        x = x + g2 * y[:B * n].reshape(B, n, D)
        if not last:
            xc = xc + g2c * y[B * n:].reshape(B, CL, D)
    return x
```

```python
import contextlib
import math
import numpy as np
import ml_dtypes
import concourse.bass as bass
import concourse.mybir as mybir
from concourse.bass import ds
from concourse.bass_utils import run_bass_kernel_spmd

F32 = mybir.dt.float32
BF16 = mybir.dt.bfloat16
I32 = mybir.dt.int32
AF = mybir.ActivationFunctionType
ALU = mybir.AluOpType
AX = mybir.AxisListType

ENGS = ("pe", "act", "dve", "pool", "sp")


class Prog:
    def __init__(self, nc, stack, n_lanes=16, n_coll=40):
        self.nc = nc
        self.items = {e: [] for e in ENGS}
        self.cnt = {e: 0 for e in ENGS}
        self.clock = {e: {} for e in ENGS}
        self.snap = {}
        self.last_w = {}
        self.readers = {}
        self.n_lanes = n_lanes
        self.lane_cnt = [0] * n_lanes
        self.lane_rr = 0
        self.n_coll = 0
        self.csem = {e: stack.enter_context(nc.semaphore("s_" + e)) for e in ENGS}
        self.lsem = [stack.enter_context(nc.semaphore("l_%d" % i)) for i in range(n_lanes)]
        self.xsem = [stack.enter_context(nc.semaphore("x_%d" % i)) for i in range(n_coll)]
        self.bsem = [stack.enter_context(nc.semaphore("b_%d" % i)) for i in range(8)]
        self.bulk_cnt = {}
        self.bulk_idx = {}
        self.n_ops = 0
        self.n_wait = 0

    def _need(self, e, ev, out):
        kind, who, val = ev
        if kind == "c" and who == e and who == "pe":
            return
        key = (kind, who)
        if self.clock[e].get(key, 0) >= val:
            return
        out[key] = max(out.get(key, 0), val)

    def _deps(self, e, reads, writes):
        need = {}
        for b in reads:
            ev = self.last_w.get(b)
            if ev is not None:
                self._need(e, ev, need)
        for b in writes:
            ev = self.last_w.get(b)
            if ev is not None:
                self._need(e, ev, need)
            for ev in self.readers.get(b, ()):
                self._need(e, ev, need)
        return need

    def _apply_waits(self, e, need):
        waits = []
        for key, val in need.items():
            if self.clock[e].get(key, 0) >= val:
                continue
            waits.append((key, val))
            self.clock[e][key] = val
            sn = self.snap.get((key[0], key[1], val))
            if sn:
                ck = self.clock[e]
                for k2, v2 in sn.items():
                    if ck.get(k2, 0) < v2:
                        ck[k2] = v2
        return waits

    def _record(self, ev, reads, writes):
        for b in reads:
            self.readers.setdefault(b, []).append(ev)
        for b in writes:
            self.last_w[b] = ev
            self.readers[b] = []

    def op(self, e, fn, reads=(), writes=()):
        need = self._deps(e, reads, writes)
        waits = self._apply_waits(e, need)
        self.cnt[e] += 1
        k = self.cnt[e]
        ev = ("c", e, k)
        if e == "pe":
            self.clock[e][("c", e)] = k
        sn = dict(self.clock[e])
        sn[("c", e)] = k
        self.snap[ev] = sn
        self.items[e].append((waits, fn, ("c", e), 1))
        self._record(ev, reads, writes)
        self.n_wait += len(waits)
        self.n_ops += 1
        return ev

    def dma(self, q, fn, reads=(), writes=()):
        need = self._deps(q, reads, writes)
        lane = self.lane_rr
        self.lane_rr = (self.lane_rr + 1) % self.n_lanes
        n_prev = self.lane_cnt[lane]
        if n_prev > 0:
            self._need(q, ("d", lane, n_prev), need)
        waits = self._apply_waits(q, need)
        self.lane_cnt[lane] += 1
        ev = ("d", lane, self.lane_cnt[lane])
        self.snap[ev] = dict(self.clock[q])
        self.items[q].append((waits, fn, ("d", lane), 16))
        self._record(ev, reads, writes)
        self.n_wait += len(waits)
        self.n_ops += 1
        return ev

    def bulk(self, q, fn, group):
        if group not in self.bulk_idx:
            self.bulk_idx[group] = len(self.bulk_idx)
            self.bulk_cnt[group] = 0
        self.bulk_cnt[group] += 1
        self.items[q].append(([], fn, ("b", self.bulk_idx[group]), 16))

    def bulk_done(self, group, key):
        ev = ("b", self.bulk_idx[group], self.bulk_cnt[group])
        self.last_w[key] = ev
        self.readers[key] = []
        return ev

    def coll(self, fn, reads=(), writes=()):
        q = "pool"
        need = self._deps(q, reads, writes)
        waits = self._apply_waits(q, need)
        idx = self.n_coll
        self.n_coll += 1
        ev = ("x", idx, 1)
        self.snap[ev] = dict(self.clock[q])
        self.items[q].append((waits, fn, ("x", idx), 1))
        self._record(ev, reads, writes)
        return ev

    def barrier(self):
        evs = []
        for lane in range(self.n_lanes):
            if self.lane_cnt[lane]:
                evs.append(("d", lane, self.lane_cnt[lane]))
        for p in ENGS:
            if self.cnt[p]:
                evs.append(("c", p, self.cnt[p]))
        for i in range(self.n_coll):
            evs.append(("x", i, 1))
        for g, gi in self.bulk_idx.items():
            evs.append(("b", gi, self.bulk_cnt[g]))
        for e in ENGS:
            need = {}
            for ev in evs:
                self._need(e, ev, need)
            waits = self._apply_waits(e, need)
            self.items[e].append((waits, None, None, 0))

    def flush(self):
        nc = self.nc

        def semof(key):
            if key[0] == "c":
                return self.csem[key[1]]
            if key[0] == "d":
                return self.lsem[key[1]]
            if key[0] == "b":
                return self.bsem[key[1]]
            return self.xsem[key[1]]

        def run(e, eng):
            for waits, fn, inc, amt in self.items[e]:
                for key, val in waits:
                    eng.wait_ge(semof(key), val * 16 if key[0] in ("d", "b") else val)
                if fn is None:
                    continue
                ins = fn(eng)
                ins.then_inc(semof(inc), amt)
            self.items[e] = []

        with nc.Block() as block:
            @block.tensor
            def _(eng):
                run("pe", eng)

            @block.scalar
            def _(eng):
                run("act", eng)

            @block.vector
            def _(eng):
                run("dve", eng)

            @block.gpsimd
            def _(eng):
                run("pool", eng)

            @block.sync
            def _(eng):
                run("sp", eng)


class Cfg:
    def __init__(self, NL=4096):
        self.D = 2048
        self.KC = 16
        self.NL = NL
        self.CL = 256
        self.NTL = NL // 128
        self.NTC = 2
        self.NT = self.NTL + self.NTC
        self.TC = NL + self.CL
        self.SEQ = 4 * NL
        self.NKEY = self.SEQ + self.CL
        self.NKT = self.NKEY // 128
        self.H = 8
        self.AW = 1024
        self.PW = 1024
        self.INW = 4096
        self.NE = 16
        self.DE = 1408
        self.JC = 11
        self.BLK = 512
        self.EPS = 1e-6
        self.DUMP0 = 3 * self.TC
        self.YROWS = 3 * self.TC + 128
        self.KB = (self.TC + self.BLK - 1) // self.BLK + 1

    def nblk(self, li):
        T = self.TC if li == 0 else self.NL
        return (2 * T) // self.BLK + self.NE

    def nblk_max(self):
        return max(self.nblk(0), self.nblk(1))


def build_program(cfg, debug=False, stop_after=None, with_moe=True):
    c = cfg
    D, KC, NL, CL, NTL, NT, TC, H = c.D, c.KC, c.NL, c.CL, c.NTL, c.NT, c.TC, c.H
    nc = bass.Bass("TRN2", target_bir_lowering=False)

    def din(name, shape, dt=F32):
        return nc.dram_tensor(name, list(shape), dt, kind="ExternalInput").ap()

    def dscr(name, shape, dt=F32):
        return nc.dram_tensor(name, list(shape), dt).ap()

    x_in = din("x_in", [NL, D])
    ctx_in = din("ctx_in", [CL, D])
    cvec = din("cvec", [2, D])
    w_mod_s = din("w_mod_s", [2, D, 6, 512])
    b_mod_s = din("b_mod_s", [2, 6, 512])
    mod_mul_s = din("mod_mul_s", [2, 6, 512])
    mod_add_s = din("mod_add_s", [2, 6, 512])
    mod_idx = din("mod_idx", [128, 1], I32)
    w_in_g = [din("w_in%d" % l, [D, c.INW]) for l in range(2)]
    q_norm = din("q_norm", [2, 64])
    k_norm = din("k_norm", [2, 64])
    lam_qk = din("lam_qk", [2, 256])
    sub_norm = din("sub_norm", [2, 128])
    w_pool = din("w_pool", [2, 4, 256, 256])
    pool_scale = din("pool_scale", [2, 128, 8])
    w_out_g = [din("w_out%d" % l, [D, D]) for l in range(2)]
    w_router = din("w_router", [D, 16])
    b_router = din("b_router", [1, 16])
    if with_moe:
        w_gate_g = [din("w_gate%d" % l, [c.NE * D, c.DE]) for l in range(2)]
        w_up_g = [din("w_up%d" % l, [c.NE * D, c.DE]) for l in range(2)]
        w_down_g = [din("w_down%d" % l, [c.NE * c.DE, D]) for l in range(2)]
    ident_d = din("ident", [128, 128])
    tri_d = din("tri", [128, 128])
    cs_tab = din("cs_tab", [TC, 64])
    pm_main = din("pm_main", [5, 4, 128, 128])
    pm_halo = din("pm_halo", [5, 4, 32, 128])
    uedge_idx = din("uedge_idx", [128, 1], I32)
    NBM = c.nblk_max()
    NFC = NT + c.KB + NBM + 176 + 11
    fconst = din("fconst", [128, NFC])
    slots_init = din("slots_init", [NBM * c.BLK, 4], I32)
    zrow = din("zrow", [1, D], BF16)
    y_out = nc.dram_tensor("y_out", [NL, D], F32, kind="ExternalOutput").ap()

    modvec = dscr("modvec", [2, 2, 6, D])
    Mg_src = dscr("Mg_src", [24, 512])
    Mg_all = dscr("Mg_all", [96, 512])
    RG4 = [[0, 1, 2, 3], [4, 5, 6, 7]]

    def w_in_rows(li, kc):
        return w_in_g[li][kc * 128:(kc + 1) * 128, :]

    def w_out_rows(li, kc):
        return w_out_g[li][kc * 128:(kc + 1) * 128, :]
    X1 = dscr("X1", [TC, D])
    XN = dscr("XN", [TC, D])
    QT = dscr("QT", [H, 128, TC], BF16)
    KT_src = dscr("KT_src", [H * 128, NL], BF16)
    KT_all = dscr("KT_all", [H, 4 * 128, NL], BF16)
    KTc = dscr("KTc", [H, 128, CL], BF16)
    V_src = dscr("V_src", [NL, 1024], BF16)
    V_all = dscr("V_all", [NL // 512, 4 * 512, 1024], BF16)
    Vc = dscr("Vc", [CL, 1024], BF16)
    U_loc = dscr("U_loc", [NL + 32, 1024])
    U_ctx = dscr("U_ctx", [CL + 32, 1024])
    Ue_src = dscr("Ue_src", [32, 1024])
    Ue_all = dscr("Ue_all", [128, 1024])
    AO = dscr("AO", [TC, 1024], BF16)
    H2 = dscr("H2", [TC + 1, D], BF16)
    slots = dscr("slots", [NBM * c.BLK, 4], I32)
    Y = dscr("Y", [c.YROWS, D])
    dbg = {}
    if debug:
        dbg["modvec"] = nc.dram_tensor("dbg_modvec", [2, 2, 6, D], F32, kind="ExternalOutput").ap()
        dbg["QT"] = nc.dram_tensor("dbg_QT", [H, 128, TC], BF16, kind="ExternalOutput").ap()
        dbg["KT_all"] = nc.dram_tensor("dbg_KT_all", [H, 4 * 128, NL], BF16, kind="ExternalOutput").ap()
        dbg["V_all"] = nc.dram_tensor("dbg_V_all", [NL // 512, 4 * 512, 1024], BF16, kind="ExternalOutput").ap()
        dbg["U_loc"] = nc.dram_tensor("dbg_U_loc", [NL + 32, 1024], F32, kind="ExternalOutput").ap()
        dbg["AO"] = nc.dram_tensor("dbg_AO", [TC, 1024], BF16, kind="ExternalOutput").ap()
        dbg["XN"] = nc.dram_tensor("dbg_XN", [TC, D], F32, kind="ExternalOutput").ap()
        dbg["H2"] = nc.dram_tensor("dbg_H2", [TC + 1, D], BF16, kind="ExternalOutput").ap()
        dbg["slots"] = nc.dram_tensor("dbg_slots", [NBM * c.BLK, 4], I32, kind="ExternalOutput").ap()
        dbg["Y"] = nc.dram_tensor("dbg_Y", [c.YROWS, D], F32, kind="ExternalOutput").ap()
        dbg["X1"] = nc.dram_tensor("dbg_X1", [TC, D], F32, kind="ExternalOutput").ap()
        dbg["bei"] = nc.dram_tensor("dbg_bei", [128, NBM], I32, kind="ExternalOutput").ap()

    top = contextlib.ExitStack()
    with top:
        P = Prog(nc, top)
        ident = top.enter_context(nc.sbuf_tensor("ident_sb", [128, 128], F32))
        identb = top.enter_context(nc.sbuf_tensor("identb", [128, 128], BF16))
        bei_top = [top.enter_context(nc.sbuf_tensor("bei_top%d" % l, [128, NBM], I32)) for l in range(2)]
        bef_top = [top.enter_context(nc.sbuf_tensor("bef_top%d" % l, [128, NBM], F32)) for l in range(2)]
        fc_top = top.enter_context(nc.sbuf_tensor("fc_top", [128, NFC], F32))
        P.dma("sp", lambda e: e.dma_start(out=fc_top[:], in_=fconst), writes=["fc_top"])
        sel_top = top.enter_context(nc.sbuf_tensor("sel_top", [128, NT, 16], F32))
        wts_top = top.enter_context(nc.sbuf_tensor("wts_top", [128, NT, 16], F32))
        P.dma("sp", lambda e: e.dma_start(out=ident[:], in_=ident_d), writes=["ident"])
        P.op("dve", lambda e: e.tensor_copy(out=identb[:], in_=ident[:]), reads=["ident"], writes=["identb"])

        phase_id = [0]

        def phase(fn):
            phase_id[0] += 1
            sfx = "_%d" % phase_id[0]
            with contextlib.ExitStack() as st:
                sb = lambda name, shape, dt=F32: st.enter_context(nc.sbuf_tensor(name + sfx, list(shape), dt))
                ps = lambda name, shape, dt=F32: st.enter_context(nc.psum_tensor(name + sfx, list(shape), dt))
                fn(sb, ps)
                P.barrier()
                P.flush()

        def dump(name, src):
            if debug and name in dbg:
                P.barrier()
                P.dma("sp", lambda e: e.dma_start(out=dbg[name], in_=src), reads=["*" + name])

        def bounce(src, dst, key, rows_per=512):
            n = src.shape[0]
            for r0 in range(0, n, rows_per):
                r1 = min(n, r0 + rows_per)
                P.bulk("act", lambda e, r0=r0, r1=r1: e.dma_start(out=dst[r0:r1, :], in_=src[r0:r1, :]), key)
            P.bulk_done(key, key)
            bounce_keys[key] = [key]

        bounce_keys = {}

        def gather4(src, dst, rkeys, wkey):
            P.coll(lambda e: e.collective_compute("AllGather", ALU.bypass, replica_groups=RG4, ins=[src.opt()], outs=[dst.opt()]),
                   reads=rkeys, writes=[wkey])

        def phase_M(sb, ps):
            pm = [ps("m_pm%d" % i, [128, 2, 512]) for i in range(2)]
            pc = ps("m_pc", [128, 2, 16])
            cs_ = sb("m_c", [16, 2, 128])
            scT = sb("m_scT", [128, 2, 16])
            scb = sb("m_scb", [128, 2, 16, 128])
            P.dma("sp", lambda e: e.dma_start(out=cs_[:], in_=cvec.rearrange("s (k p) -> k s p", p=128)), writes=["m_c"])
            for s_ in range(2):
                P.op("pe", lambda e, s_=s_: e.transpose(out=pc[:, s_, :], in_=cs_[:, s_, :], identity=ident[0:16, 0:16]),
                     reads=["m_c", "ident"], writes=["m_pc"])
            P.op("act", lambda e: e.activation(out=scT[:], in_=pc[:], func=AF.Silu), reads=["m_pc"], writes=["m_scT"])
            P.op("dve", lambda e: e.tensor_copy(out=scb[:], in_=scT[:].unsqueeze(3).to_broadcast([128, 2, 16, 128])),
                 reads=["m_scT"], writes=["m_scb"])
            wch = [sb("m_w%d" % i, [128, 16, 512]) for i in range(2)]
            bma = [sb("m_bma%d" % i, [1, 3, 512]) for i in range(2)]
            res = sb("m_r", [1, 2, 2, 6, 512])
            it = 0
            for li in range(2):
                for w in range(6):
                    b = it % 2
                    it += 1
                    P.dma("sp", lambda e, b=b, li=li, w=w: e.dma_start(
                        out=wch[b][:], in_=w_mod_s[li, :, w, :].rearrange("(k p) n -> p k n", p=128)), writes=["m_w%d" % b])
                    for jj, srcv in enumerate((b_mod_s, mod_add_s, mod_mul_s)):
                        P.dma("sp", lambda e, b=b, li=li, w=w, jj=jj, srcv=srcv: e.dma_start(out=bma[b][0:1, jj, :], in_=srcv[li, w:w + 1, :]),
                              writes=["m_bma%d" % b])
                    for s_ in range(2):
                        for kc in range(KC):
                            P.op("pe", lambda e, b=b, s_=s_, kc=kc: e.matmul(pm[b][:, s_, :], lhsT=scb[:, s_, kc, :], rhs=wch[b][:, kc, :],
                                                                            start=(kc == 0), stop=(kc == KC - 1)),
                                 reads=["m_scb", "m_w%d" % b], writes=["m_pm%d" % b])
                    for s_ in range(2):
                        rv = res[0:1, li, s_, w, :]
                        P.op("dve", lambda e, b=b, s_=s_, rv=rv: e.tensor_tensor(out=rv, in0=pm[b][0:1, s_, :], in1=bma[b][0:1, 0, :], op=ALU.add),
                             reads=["m_pm%d" % b, "m_bma%d" % b], writes=["m_r"])
                        P.op("dve", lambda e, b=b, rv=rv: e.tensor_tensor(out=rv, in0=rv, in1=bma[b][0:1, 1, :], op=ALU.add),
                             reads=["m_r", "m_bma%d" % b], writes=["m_r"])
                        P.op("dve", lambda e, b=b, rv=rv: e.tensor_tensor(out=rv, in0=rv, in1=bma[b][0:1, 2, :], op=ALU.mult),
                             reads=["m_r", "m_bma%d" % b], writes=["m_r"])
            P.dma("sp", lambda e: e.dma_start(out=Mg_src.rearrange("(o a) n -> o a n", o=1), in_=res[0:1].rearrange("o l s w n -> o (l s w) n")),
                  reads=["m_r"], writes=["*Mg_src"])
            gather4(Mg_src, Mg_all, ["*Mg_src"], "*Mg_all")
            mi = sb("m_mi", [128, 1], I32)
            mrow = sb("m_mrow", [128, 512])
            P.dma("sp", lambda e: e.dma_start(out=mi[:], in_=mod_idx), writes=["m_mi"])
            mvr = modvec.rearrange("l s w (r n) -> (l s w r) n", n=512)
            P.dma("pool", lambda e: e.indirect_dma_start(out=mrow[:], out_offset=None, in_=Mg_all,
                                                         in_offset=bass.IndirectOffsetOnAxis(ap=mi[:, 0:1], axis=0)),
                  reads=["m_mi", "*Mg_all"], writes=["m_mrow"])
            P.dma("sp", lambda e: e.dma_start(out=mvr[0:96, :], in_=mrow[0:96, :]), reads=["m_mrow"], writes=["*modvec"])
            dump("modvec", modvec)

        def load_bvec(dst, src_row, key, q="sp"):
            P.dma(q, lambda e: e.dma_start(out=dst, in_=src_row.partition_broadcast(128).rearrange("p o n -> p (o n)")),
                  reads=["*modvec"], writes=[key])

        def rms_mod(xt, key_x, hout, key_h, Ab, Sb, keyA, tmp):
            sq, ss, rs = tmp["sq"], tmp["ss"], tmp["rs"]
            P.op("act", lambda e: e.activation(out=sq[:], in_=xt[:], func=AF.Square, accum_out=ss[:]),
                 reads=[key_x], writes=["sq", "ss"])
            P.op("act", lambda e: e.activation(out=rs[:], in_=ss[:], func=AF.Sqrt, scale=1.0 / D, bias=c.EPS),
                 reads=["ss"], writes=["rs"])
            P.op("dve", lambda e: e.reciprocal(out=rs[:], in_=rs[:]), reads=["rs"], writes=["rs"])
            P.op("dve", lambda e: e.scalar_tensor_tensor(out=xt[:], in0=xt[:], scalar=rs[:], in1=Ab[:], op0=ALU.mult, op1=ALU.mult),
                 reads=[key_x, "rs", keyA], writes=[key_x])
            P.op("pool", lambda e: e.tensor_tensor(out=hout[:], in0=xt[:], in1=Sb[:], op=ALU.add),
                 reads=[key_x, keyA], writes=[key_h])

        def x_src(li, i):
            if li == 0:
                if i < NTL:
                    return x_in[i * 128:(i + 1) * 128, :]
                return ctx_in[(i - NTL) * 128:(i - NTL + 1) * 128, :]
            return X1[i * 128:(i + 1) * 128, :]

        def phase_A(li, half):
            def body(sb, ps):
                wst = sb("a_wst", [128, 2048])
                wbf = sb("a_wbf", [128, 16, 2048], BF16)
                Ab = sb("a_Ab", [128, D])
                Sb = sb("a_Sb", [128, D])
                xt = [sb("a_xt%d" % i, [128, D]) for i in range(2)]
                hb = sb("a_h", [128, D], BF16)
                hT = [sb("a_hT%d" % i, [128, 16, 128], BF16) for i in range(2)]
                tmp = {"sq": sb("a_sq", [128, D], BF16), "ss": sb("a_ss", [128, 1]), "rs": sb("a_rs", [128, 1])}
                pT = ps("a_pT", [128, 16, 128], BF16)
                pp = [ps("a_pp%d" % i, [128, 1024]) for i in range(2)]
                for kc in range(KC):
                    P.dma("sp", lambda e, kc=kc: e.dma_start(out=wst[:], in_=w_in_rows(li, kc)[:, half * 2048:(half + 1) * 2048]),
                          writes=["a_wst"])
                    P.op("dve" if kc % 2 == 0 else "pool", lambda e, kc=kc: e.tensor_copy(out=wbf[:, kc, :], in_=wst[:]),
                         reads=["a_wst"], writes=["a_wbf"])
                if half == 0:
                    cst = sb("a_cs", [128, NT, 64])
                    P.dma("sp", lambda e: e.dma_start(out=cst[:], in_=cs_tab.rearrange("(t p) f -> p t f", p=128)), writes=["a_cs"])
                    gq = sb("a_gq", [128, 64])
                    gk = sb("a_gk", [128, 64])
                    load_bvec(gq[:], q_norm[li:li + 1, :], "a_gq")
                    load_bvec(gk[:], k_norm[li:li + 1, :], "a_gk")
                    P.op("dve", lambda e: e.tensor_scalar(out=gq[:], in0=gq[:], scalar1=0.125, scalar2=None, op0=ALU.mult),
                         reads=["a_gq"], writes=["a_gq"])
                    sqq = sb("a_sqq", [128, 1024])
                    ssq = sb("a_ssq", [128, 16])
                    rq = sb("a_rq", [128, 16])
                    qn = sb("a_qn", [128, 1024])
                    tA = sb("a_tA", [128, 512])
                    tB = sb("a_tB", [128, 512])
                    qr = sb("a_qr", [128, 1024], BF16)
                    qTs = [sb("a_qTs%d" % i, [128, 8, 128], BF16) for i in range(2)]
                    pq = ps("a_pq", [128, 8, 128], BF16)
                else:
                    vs = sb("a_vs", [128, 1024], BF16)
                    us = sb("a_us", [128, 1024])
                    zt = sb("a_z", [128, 1024])
                    if li == 0:
                        P.op("dve", lambda e: e.memset(zt[:], 0.0), writes=["a_z"])
                        P.dma("sp", lambda e: e.dma_start(out=U_loc[0:16, :], in_=zt[0:16, :]), reads=["a_z"], writes=["*U"])
                        P.dma("sp", lambda e: e.dma_start(out=U_loc[NL + 16:NL + 32, :], in_=zt[0:16, :]), reads=["a_z"], writes=["*U"])
                        P.dma("sp", lambda e: e.dma_start(out=U_ctx[0:16, :], in_=zt[0:16, :]), reads=["a_z"], writes=["*U"])
                        P.dma("sp", lambda e: e.dma_start(out=U_ctx[CL + 16:CL + 32, :], in_=zt[0:16, :]), reads=["a_z"], writes=["*U"])
                cur_mod = [None]
                for i in range(NT):
                    isctx = i >= NTL
                    src = 1 if isctx else 0
                    if cur_mod[0] != src:
                        cur_mod[0] = src
                        load_bvec(Ab[:], modvec[li, src, 1:2, :], "a_mod")
                        load_bvec(Sb[:], modvec[li, src, 0:1, :], "a_mod")
                    xb = i % 2
                    kx = "a_xt%d" % xb
                    P.dma("sp", lambda e, i=i, xb=xb: e.dma_start(out=xt[xb][:], in_=x_src(li, i)), reads=["*X1"], writes=[kx])
                    rms_mod(xt[xb], kx, hb, "a_h", Ab, Sb, "a_mod", tmp)
                    for kc in range(KC):
                        P.op("pe", lambda e, kc=kc: e.transpose(out=pT[:, kc, :], in_=hb[:, kc * 128:(kc + 1) * 128], identity=identb[:]),
                             reads=["a_h", "identb"], writes=["a_pT"])
                    P.op("act", lambda e, xb=xb: e.copy(out=hT[xb][:], in_=pT[:]), reads=["a_pT"], writes=["a_hT%d" % xb])
                    for part in range(2):
                        if half == 0 and part == 0 and isctx and li == 1:
                            continue
                        if half == 1 and part == 1 and isctx and li == 1:
                            continue
                        pb = pp[part]
                        kp = "a_pp%d" % part
                        for g in range(2):
                            for kc in range(KC):
                                P.op("pe", lambda e, g=g, kc=kc, xb=xb, part=part, pb=pb: e.matmul(
                                    pb[:, g * 512:(g + 1) * 512], lhsT=hT[xb][:, kc, :],
                                    rhs=wbf[:, kc, part * 1024 + g * 512: part * 1024 + (g + 1) * 512],
                                    start=(kc == 0), stop=(kc == KC - 1)),
                                    reads=["a_hT%d" % xb, "a_wbf"], writes=[kp])
                        if half == 0:
                            gain = gq if part == 0 else gk
                            kg = "a_gq" if part == 0 else "a_gk"
                            P.op("act", lambda e, pb=pb: e.activation(out=sqq[:], in_=pb[:], func=AF.Square), reads=[kp], writes=["a_sqq"])
                            P.op("dve", lambda e: e.tensor_reduce(out=ssq[:], in_=sqq[:].rearrange("p (g d) -> p g d", d=64), axis=AX.X, op=ALU.add),
                                 reads=["a_sqq"], writes=["a_ssq"])
                            P.op("act", lambda e: e.activation(out=rq[:], in_=ssq[:], func=AF.Sqrt, scale=1.0 / 64, bias=c.EPS),
                                 reads=["a_ssq"], writes=["a_rq"])
                            P.op("dve", lambda e: e.reciprocal(out=rq[:], in_=rq[:]), reads=["a_rq"], writes=["a_rq"])
                            P.op("dve", lambda e, pb=pb: e.tensor_tensor(
                                out=qn[:].rearrange("p (g d) -> p g d", d=64), in0=pb[:].rearrange("p (g d) -> p g d", d=64),
                                in1=rq[:].unsqueeze(2).to_broadcast([128, 16, 64]), op=ALU.mult),
                                reads=[kp, "a_rq"], writes=["a_qn"])
                            P.op("pool", lambda e, gain=gain: e.tensor_tensor(
                                out=qn[:].rearrange("p (g d) -> p g d", d=64), in0=qn[:].rearrange("p (g d) -> p g d", d=64),
                                in1=gain[:].unsqueeze(1).to_broadcast([128, 16, 64]), op=ALU.mult),
                                reads=["a_qn", kg], writes=["a_qn"])
                            qv = qn[:].rearrange("p (hm ax hf nf) -> p hm ax hf nf", hm=16, ax=2, hf=2)
                            qo = qr[:].rearrange("p (hm ax hf nf) -> p hm ax hf nf", hm=16, ax=2, hf=2)
                            x1, x2 = qv[:, :, :, 0, :], qv[:, :, :, 1, :]
                            cosb = cst[:, i, 0:32].rearrange("p (ax nf) -> p ax nf", ax=2).unsqueeze(1).to_broadcast([128, 16, 2, 16])
                            sinb = cst[:, i, 32:64].rearrange("p (ax nf) -> p ax nf", ax=2).unsqueeze(1).to_broadcast([128, 16, 2, 16])
                            tAv = tA[:].rearrange("p (hm ax nf) -> p hm ax nf", hm=16, ax=2)
                            tBv = tB[:].rearrange("p (hm ax nf) -> p hm ax nf", hm=16, ax=2)
                            P.op("dve", lambda e, x1=x1, cosb=cosb, tAv=tAv: e.tensor_tensor(out=tAv, in0=x1, in1=cosb, op=ALU.mult),
                                 reads=["a_qn", "a_cs"], writes=["a_tA"])
                            P.op("pool", lambda e, x2=x2, sinb=sinb, tBv=tBv: e.tensor_tensor(out=tBv, in0=x2, in1=sinb, op=ALU.mult),
                                 reads=["a_qn", "a_cs"], writes=["a_tB"])
                            P.op("dve", lambda e, qo=qo, tAv=tAv, tBv=tBv: e.tensor_tensor(out=qo[:, :, :, 0, :], in0=tAv, in1=tBv, op=ALU.subtract),
                                 reads=["a_tA", "a_tB"], writes=["a_qr"])
                            P.op("pool", lambda e, x2=x2, cosb=cosb, tAv=tAv: e.tensor_tensor(out=tAv, in0=x2, in1=cosb, op=ALU.mult),
                                 reads=["a_qn", "a_cs"], writes=["a_tA"])
                            P.op("dve", lambda e, x1=x1, sinb=sinb, tBv=tBv: e.tensor_tensor(out=tBv, in0=x1, in1=sinb, op=ALU.mult),
                                 reads=["a_qn", "a_cs"], writes=["a_tB"])
                            P.op("pool", lambda e, qo=qo, tAv=tAv, tBv=tBv: e.tensor_tensor(out=qo[:, :, :, 1, :], in0=tAv, in1=tBv, op=ALU.add),
                                 reads=["a_tA", "a_tB"], writes=["a_qr"])
                            for hh in range(H):
                                P.op("pe", lambda e, hh=hh: e.transpose(out=pq[:, hh, :], in_=qr[:, hh * 128:(hh + 1) * 128], identity=identb[:]),
                                     reads=["a_qr", "identb"], writes=["a_pq"])
                            qb_ = part
                            P.op("act", lambda e, qb_=qb_: e.copy(out=qTs[qb_][:], in_=pq[:]), reads=["a_pq"], writes=["a_qTs%d" % qb_])
                            if part == 0:
                                dst = QT[:, :, i * 128:(i + 1) * 128].rearrange("h p t -> p h t")
                                wk = "*QT"
                            elif not isctx:
                                dst = KT_src.rearrange("(h p) t -> p h t", p=128)[:, :, i * 128:(i + 1) * 128]
                                wk = "*KT_src"
                            else:
                                dst = KTc[:, :, (i - NTL) * 128:(i - NTL + 1) * 128].rearrange("h p t -> p h t")
                                wk = "*KTc"
                            P.dma("sp", lambda e, dst=dst, qb_=qb_: e.dma_start(out=dst, in_=qTs[qb_][:]), reads=["a_qTs%d" % qb_], writes=[wk])
                        else:
                            if part == 0:
                                P.op("act", lambda e, pb=pb: e.copy(out=vs[:], in_=pb[:]), reads=[kp], writes=["a_vs"])
                                dst = V_src[i * 128:(i + 1) * 128, :] if not isctx else Vc[(i - NTL) * 128:(i - NTL + 1) * 128, :]
                                P.dma("sp", lambda e, dst=dst: e.dma_start(out=dst, in_=vs[:]), reads=["a_vs"],
                                      writes=["*V_src" if not isctx else "*Vc"])
                            else:
                                P.op("dve", lambda e, pb=pb: e.tensor_copy(out=us[:], in_=pb[:]), reads=[kp], writes=["a_us"])
                                if not isctx:
                                    dst = U_loc[16 + i * 128:16 + (i + 1) * 128, :]
                                else:
                                    dst = U_ctx[16 + (i - NTL) * 128:16 + (i - NTL + 1) * 128, :]
                                P.dma("sp", lambda e, dst=dst: e.dma_start(out=dst, in_=us[:]), reads=["a_us"], writes=["*U"])
                                if i == 0:
                                    P.dma("sp", lambda e: e.dma_start(out=Ue_src[0:16, :], in_=us[0:16, :]), reads=["a_us"], writes=["*Ue_src"])
                                if i == NTL - 1:
                                    P.dma("sp", lambda e: e.dma_start(out=Ue_src[16:32, :], in_=us[112:128, :]), reads=["a_us"], writes=["*Ue_src"])
            phase(body)

        def phase_G(li):
            def body(sb, ps):
                for hh in range(H):
                    gather4(KT_src[hh * 128:(hh + 1) * 128, :], KT_all[hh], ["*KT_src"], "*KT_all%d" % hh)
                for j in range(NL // 512):
                    gather4(V_src[j * 512:(j + 1) * 512, :], V_all[j], ["*V_src"], "*V_all%d" % j)
                gather4(Ue_src, Ue_all, ["*Ue_src"], "*Ue_all")
                ui = sb("g_ui", [128, 1], I32)
                um = sb("g_um", [128, 1024])
                P.dma("sp", lambda e: e.dma_start(out=ui[:], in_=uedge_idx), writes=["g_ui"])
                P.dma("pool", lambda e: e.indirect_dma_start(out=um[:], out_offset=None, in_=Ue_all,
                                                             in_offset=bass.IndirectOffsetOnAxis(ap=ui[:, 0:1], axis=0)),
                      reads=["g_ui", "*Ue_all"], writes=["g_um"])
                P.dma("sp", lambda e: e.dma_start(out=U_loc[0:16, :], in_=um[0:16, :]), reads=["g_um"], writes=["*U"])
                P.dma("sp", lambda e: e.dma_start(out=U_loc[NL + 16:NL + 32, :], in_=um[16:32, :]), reads=["g_um"], writes=["*U"])
            phase(body)
            dump("QT", QT)
            dump("KT_all", KT_all)
            dump("V_all", V_all)
            dump("U_loc", U_loc)

        def phase_B(li):
            lam_init = 0.8 - 0.6 * math.exp(-0.3 * li)

            def body(sb, ps):
                NKT, NKEY = c.NKT, c.NKEY
                kT = [sb("b_kT%d" % i, [128, NKEY], BF16) for i in range(2)]
                vA = [sb("b_vA%d" % i, [128, NKT, 129], BF16) for i in range(2)]
                qT = [sb("b_qT%d" % i, [128, TC], BF16) for i in range(2)]
                pT = [sb("b_pT%d" % i, [128, 1024], BF16) for i in range(2)]
                sT = [ps("b_sT%d" % i, [128, 1024]) for i in range(2)]
                Ob = [ps("b_O%d" % i, [128, 512]) for i in range(3)]

                def acc(a):
                    return Ob[a // 3][:, (a % 3) * 129:(a % 3) * 129 + 129]
                lv = sb("b_lv", [128, 256])
                lt = sb("b_lt", [128, 128])
                ls = sb("b_ls", [128, 2])
                nlam = sb("b_nlam", [128, 1])
                gsub = sb("b_gsub", [128, 128])
                r12 = sb("b_r12", [128, 8])
                ot = sb("b_ot", [128, 4, 128])
                osq = sb("b_osq", [128, 4, 128])
                oss = sb("b_oss", [128, 4])
                orstd = sb("b_orstd", [128, 4])
                aos = [sb("b_aos%d" % i, [128, 4, 128], BF16) for i in range(2)]
                load_bvec(lv[:], lam_qk[li:li + 1, :], "b_lv")
                P.op("dve", lambda e: e.tensor_tensor(out=lt[:].rearrange("p (a d) -> p a d", a=2),
                                                      in0=lv[:].rearrange("p (a b d) -> p a b d", a=2, b=2)[:, :, 0, :],
                                                      in1=lv[:].rearrange("p (a b d) -> p a b d", a=2, b=2)[:, :, 1, :], op=ALU.mult),
                     reads=["b_lv"], writes=["b_lt"])
                P.op("dve", lambda e: e.tensor_reduce(out=ls[:], in_=lt[:].rearrange("p (a d) -> p a d", a=2), axis=AX.X, op=ALU.add),
                     reads=["b_lt"], writes=["b_ls"])
                P.op("act", lambda e: e.activation(out=ls[:], in_=ls[:], func=AF.Exp), reads=["b_ls"], writes=["b_ls"])
                P.op("dve", lambda e: e.tensor_tensor(out=nlam[:], in0=ls[:, 1:2], in1=ls[:, 0:1], op=ALU.subtract), reads=["b_ls"], writes=["b_nlam"])
                P.op("dve", lambda e: e.tensor_scalar(out=nlam[:], in0=nlam[:], scalar1=-lam_init, scalar2=None, op0=ALU.add),
                     reads=["b_nlam"], writes=["b_nlam"])
                load_bvec(gsub[:], sub_norm[li:li + 1, :], "b_gsub")
                P.op("dve", lambda e: e.tensor_scalar(out=gsub[:], in0=gsub[:], scalar1=1.0 - lam_init, scalar2=None, op0=ALU.mult),
                     reads=["b_gsub"], writes=["b_gsub"])
                for i in range(2):
                    P.op("pool", lambda e, i=i: e.memset(vA[i][:, :, 128:129], 1.0), writes=["b_vA%d" % i])
                it = [0]
                blkc = [0]
                for h in range(H):
                    hb_ = h % 2
                    kk, kv, kq = "b_kT%d" % hb_, "b_vA%d" % hb_, "b_qT%d" % hb_
                    for rk in range(4):
                        P.dma("sp", lambda e, rk=rk, h=h, hb_=hb_: e.dma_start(out=kT[hb_][:, rk * NL:(rk + 1) * NL],
                                                                               in_=KT_all[h, rk * 128:(rk + 1) * 128, :]),
                              reads=["*KT_all%d" % h], writes=[kk])
                        for j in range(NL // 512):
                            P.dma("sp", lambda e, rk=rk, h=h, hb_=hb_, j=j: e.dma_start(
                                out=vA[hb_][:, rk * NTL + j * 4:rk * NTL + j * 4 + 4, 0:128],
                                in_=V_all[j, rk * 512:(rk + 1) * 512, h * 128:(h + 1) * 128].rearrange("(t p) c -> p t c", p=128)),
                                reads=["*V_all%d" % j], writes=[kv])
                    P.dma("sp", lambda e, h=h, hb_=hb_: e.dma_start(out=kT[hb_][:, 4 * NL:4 * NL + CL], in_=KTc[h]), reads=["*KTc"], writes=[kk])
                    P.dma("sp", lambda e, h=h, hb_=hb_: e.dma_start(
                        out=vA[hb_][:, 4 * NTL:4 * NTL + 2, 0:128], in_=Vc[:, h * 128:(h + 1) * 128].rearrange("(t p) c -> p t c", p=128)),
                        reads=["*Vc"], writes=[kv])
                    P.dma("sp", lambda e, h=h, hb_=hb_: e.dma_start(out=qT[hb_][:], in_=QT[h]), reads=["*QT"], writes=[kq])
                    blocks = [(qb * 512, 512, list(range(NKT))) for qb in range(NL // 512)]
                    if li == 0:
                        blocks.append((NL, CL, [4 * NTL, 4 * NTL + 1]))
                    for (q0, nq, kts) in blocks:
                        nsub = nq // 128
                        for ki, kt in enumerate(kts):
                            sb_ = it[0] % 2
                            it[0] += 1
                            ks, kp_ = "b_sT%d" % sb_, "b_pT%d" % sb_
                            for m in range(2):
                                P.op("pe", lambda e, m=m, kt=kt, q0=q0, nq=nq, sb_=sb_, hb_=hb_: e.matmul(
                                    sT[sb_][:, m * 512:m * 512 + nq], lhsT=kT[hb_][m * 64:(m + 1) * 64, kt * 128:(kt + 1) * 128],
                                    rhs=qT[hb_][m * 64:(m + 1) * 64, q0:q0 + nq], start=True, stop=True),
                                    reads=[kk, kq], writes=[ks])
                            if nq == 512:
                                P.op("act", lambda e, sb_=sb_: e.activation(out=pT[sb_][:], in_=sT[sb_][:], func=AF.Exp), reads=[ks], writes=[kp_])
                            else:
                                P.op("act", lambda e, sb_=sb_, nq=nq: e.activation(
                                    out=pT[sb_][:].rearrange("p (m q) -> p m q", m=2)[:, :, 0:nq],
                                    in_=sT[sb_][:].rearrange("p (m q) -> p m q", m=2)[:, :, 0:nq], func=AF.Exp), reads=[ks], writes=[kp_])
                            if ki == 0:
                                started = set()
                            for m in range(2):
                                for j in range(nsub):
                                    a = m * 4 + j
                                    first_in_bank = (ki == 0) and ((a // 3) not in started)
                                    started.add(a // 3)
                                    P.op("pe", lambda e, a=a, m=m, j=j, kt=kt, sb_=sb_, hb_=hb_, ki=ki, fb=first_in_bank: e.matmul(
                                        acc(a), lhsT=pT[sb_][:, m * 512 + j * 128:m * 512 + (j + 1) * 128], rhs=vA[hb_][:, kt, :],
                                        start=fb, stop=(ki == len(kts) - 1), skip_group_check=True),
                                        reads=[kp_, kv], writes=["b_O"])
                        for j in range(nsub):
                            for m in range(2):
                                a = m * 4 + j
                                P.op("dve", lambda e, a=a, m=m, j=j: e.reciprocal(out=r12[:, m * 4 + j:m * 4 + j + 1], in_=acc(a)[:, 128:129]),
                                     reads=["b_O"], writes=["b_r12"])
                        P.op("dve", lambda e: e.tensor_scalar(out=r12[:, 4:8], in0=r12[:, 4:8], scalar1=nlam[:, 0:1], scalar2=None, op0=ALU.mult),
                             reads=["b_r12", "b_nlam"], writes=["b_r12"])
                        for j in range(nsub):
                            P.op("dve", lambda e, j=j: e.tensor_scalar(out=ot[:, j, :], in0=acc(j)[:, 0:128], scalar1=r12[:, j:j + 1], scalar2=None, op0=ALU.mult),
                                 reads=["b_O", "b_r12"], writes=["b_ot"])
                            P.op("dve", lambda e, j=j: e.scalar_tensor_tensor(out=ot[:, j, :], in0=acc(4 + j)[:, 0:128], scalar=r12[:, 4 + j:5 + j],
                                                                              in1=ot[:, j, :], op0=ALU.mult, op1=ALU.add),
                                 reads=["b_O", "b_r12", "b_ot"], writes=["b_ot"])
                        P.op("pool", lambda e, nsub=nsub: e.tensor_tensor(out=osq[:, 0:nsub, :], in0=ot[:, 0:nsub, :], in1=ot[:, 0:nsub, :], op=ALU.mult),
                             reads=["b_ot"], writes=["b_osq"])
                        P.op("dve", lambda e, nsub=nsub: e.tensor_reduce(out=oss[:, 0:nsub], in_=osq[:, 0:nsub, :], axis=AX.X, op=ALU.add),
                             reads=["b_osq"], writes=["b_oss"])
                        P.op("act", lambda e, nsub=nsub: e.activation(out=oss[:, 0:nsub], in_=oss[:, 0:nsub], func=AF.Ln, scale=1.0 / 128, bias=c.EPS),
                             reads=["b_oss"], writes=["b_oss"])
                        P.op("act", lambda e, nsub=nsub: e.activation(out=orstd[:, 0:nsub], in_=oss[:, 0:nsub], func=AF.Exp, scale=-0.5),
                             reads=["b_oss"], writes=["b_orstd"])
                        ab_ = blkc[0] % 2
                        blkc[0] += 1
                        for j in range(nsub):
                            P.op("dve", lambda e, j=j, ab_=ab_: e.scalar_tensor_tensor(out=aos[ab_][:, j, :], in0=ot[:, j, :], scalar=orstd[:, j:j + 1],
                                                                                     in1=gsub[:], op0=ALU.mult, op1=ALU.mult),
                                 reads=["b_ot", "b_orstd", "b_gsub"], writes=["b_aos%d" % ab_])
                        P.dma("sp", lambda e, q0=q0, nq=nq, nsub=nsub, h=h, ab_=ab_: e.dma_start(
                            out=AO[q0:q0 + nq, h * 128:(h + 1) * 128].rearrange("(j p) c -> p j c", p=128), in_=aos[ab_][:, 0:nsub, :]),
                            reads=["b_aos%d" % ab_], writes=["*AO"])
            phase(body)
            dump("AO", AO)

        def phase_C(li):
            NTr = NT if li == 0 else NTL
            Tr = NTr * 128
            NB = c.nblk(li)
            state = {}

            def body1(sb, ps):
                wst = sb("c_wst", [128, 2048])
                wo = sb("c_wo", [128, 16, 2048], BF16)
                wpst = wst[:].rearrange("p (a d) -> p a d", a=8)
                wp = sb("c_wp", [128, 8, 256], BF16)
                pmm = sb("c_pmm", [128, 20, 128])
                pmh = sb("c_pmh", [32, 20, 128])
                psc = sb("c_psc", [128, 8])
                wr = sb("c_wr", [128, 16, 16])
                brb = sb("c_brb", [128, 16])
                G1b = sb("c_G1", [128, D])
                A2b = sb("c_A2", [128, D])
                S2b = sb("c_S2", [128, D])
                xt = [sb("c_xt%d" % i, [128, D]) for i in range(2)]
                h2 = sb("c_h2", [128, D])
                h2b = sb("c_h2b", [128, D], BF16)
                h2T = sb("c_h2T", [128, 16, 128])
                ao = sb("c_ao", [128, 1024], BF16)
                us = sb("c_us", [128, 1024])
                uh = sb("c_uh", [32, 1024])
                catT = sb("c_catT", [128, 16, 128], BF16)
                dTs = sb("c_dTs", [128, 8, 128], BF16)
                tmp = {"sq": sb("c_sq", [128, D], BF16), "ss": sb("c_ss", [128, 1]), "rs": sb("c_rs", [128, 1])}
                sel_all, wts_all = sel_top, wts_top
                lg = sb("c_lg", [128, 16])
                ee = sb("c_ee", [128, 16])
                e2 = sb("c_e2", [128, 16])
                m1 = sb("c_m1", [128, 4])
                m2 = sb("c_m2", [128, 4])
                gs = sb("c_gs", [128, 4])
                sc1 = sb("c_sc1", [128, 4])
                pbig = ps("c_pbig", [128, 2048])
                pmid = ps("c_pmid", [128, 8, 128])
                pao = ps("c_pao", [128, 8, 128], BF16)
                plog = ps("c_plog", [128, 16])
                for kc in range(KC):
                    P.dma("sp", lambda e, kc=kc: e.dma_start(out=wst[:], in_=w_out_rows(li, kc)), writes=["c_wst"])
                    P.op("dve" if kc % 2 == 0 else "pool", lambda e, kc=kc: e.tensor_copy(out=wo[:, kc, :], in_=wst[:]),
                         reads=["c_wst"], writes=["c_wo"])
                P.dma("sp", lambda e: e.dma_start(out=wpst, in_=w_pool[li].rearrange("g (cc p) d -> p (g cc) d", p=128)), writes=["c_wst"])
                P.op("dve", lambda e: e.tensor_copy(out=wp[:], in_=wpst), reads=["c_wst"], writes=["c_wp"])
                P.dma("sp", lambda e: e.dma_start(out=pmm[:], in_=pm_main.rearrange("k g p t -> p (k g) t")), writes=["c_pmm"])
                P.dma("sp", lambda e: e.dma_start(out=pmh[:], in_=pm_halo.rearrange("k g p t -> p (k g) t")), writes=["c_pmh"])
                P.dma("sp", lambda e: e.dma_start(out=psc[:], in_=pool_scale[li]), writes=["c_psc"])
                P.dma("sp", lambda e: e.dma_start(out=wr[:], in_=w_router.rearrange("(k p) n -> p k n", p=128)), writes=["c_wr"])
                load_bvec(brb[:], b_router[0:1, :], "c_brb")
                cur_mod = [None]
                for i in range(NT):
                    isctx = i >= NTL
                    if isctx and li == 1:
                        continue
                    src = 1 if isctx else 0
                    if cur_mod[0] != src:
                        cur_mod[0] = src
                        load_bvec(G1b[:], modvec[li, src, 2:3, :], "c_mod")
                        load_bvec(A2b[:], modvec[li, src, 4:5, :], "c_mod")
                        load_bvec(S2b[:], modvec[li, src, 3:4, :], "c_mod")
                    if not isctx:
                        cls = 0 if i == 0 else (2 if i == NTL - 1 else 1)
                        ub = U_loc[16 + i * 128:16 + (i + 1) * 128, :]
                        uh0 = U_loc[i * 128:i * 128 + 16, :]
                        uh1 = U_loc[16 + (i + 1) * 128:32 + (i + 1) * 128, :]
                    else:
                        ic = i - NTL
                        cls = 3 + ic
                        ub = U_ctx[16 + ic * 128:16 + (ic + 1) * 128, :]
                        uh0 = U_ctx[ic * 128:ic * 128 + 16, :]
                        uh1 = U_ctx[16 + (ic + 1) * 128:32 + (ic + 1) * 128, :]
                    xb = i % 2
                    kx = "c_xt%d" % xb
                    P.dma("sp", lambda e, i=i, xb=xb: e.dma_start(out=xt[xb][:], in_=x_src(li, i)), reads=["*X1"], writes=[kx])
                    P.dma("sp", lambda e, i=i: e.dma_start(out=ao[:], in_=AO[i * 128:(i + 1) * 128, :]), reads=["*AO"], writes=["c_ao"])
                    P.dma("sp", lambda e, ub=ub: e.dma_start(out=us[:], in_=ub), reads=["*U"], writes=["c_us"])
                    P.dma("sp", lambda e, uh0=uh0: e.dma_start(out=uh[0:16, :], in_=uh0), reads=["*U"], writes=["c_uh"])
                    P.dma("sp", lambda e, uh1=uh1: e.dma_start(out=uh[16:32, :], in_=uh1), reads=["*U"], writes=["c_uh"])
                    for hh in range(H):
                        P.op("pe", lambda e, hh=hh: e.transpose(out=pao[:, hh, :], in_=ao[:, hh * 128:(hh + 1) * 128], identity=identb[:]),
                             reads=["c_ao", "identb"], writes=["c_pao"])
                    P.op("act", lambda e: e.copy(out=catT[:, 0:8, :], in_=pao[:]), reads=["c_pao"], writes=["c_catT"])
                    for cc in range(8):
                        g = cc // 2
                        P.op("pe", lambda e, cc=cc, g=g, cls=cls: e.matmul(pmid[:, cc, :], lhsT=us[:, cc * 128:(cc + 1) * 128],
                                                                           rhs=pmm[:, cls * 4 + g, :], start=True, stop=False),
                             reads=["c_us", "c_pmm"], writes=["c_pmid"])
                        P.op("pe", lambda e, cc=cc, g=g, cls=cls: e.matmul(pmid[:, cc, :], lhsT=uh[:, cc * 128:(cc + 1) * 128],
                                                                           rhs=pmh[:, cls * 4 + g, :], start=False, stop=True),
                             reads=["c_uh", "c_pmh"], writes=["c_pmid"])
                    P.op("dve", lambda e: e.tensor_copy(out=dTs[:], in_=pmid[:]), reads=["c_pmid"], writes=["c_dTs"])
                    for dc in range(8):
                        g, dd = dc // 2, dc % 2
                        for cc in range(2):
                            P.op("pe", lambda e, dc=dc, g=g, dd=dd, cc=cc: e.matmul(pmid[:, dc, :], lhsT=wp[:, g * 2 + cc, dd * 128:(dd + 1) * 128],
                                                                                   rhs=dTs[:, g * 2 + cc, :], start=(cc == 0), stop=(cc == 1)),
                                 reads=["c_wp", "c_dTs"], writes=["c_pmid"])
                    P.op("dve", lambda e: e.tensor_tensor(out=catT[:, 8:16, :], in0=pmid[:], in1=psc[:].unsqueeze(2).to_broadcast([128, 8, 128]), op=ALU.mult),
                         reads=["c_pmid", "c_psc"], writes=["c_catT"])
                    for ng in range(4):
                        for f in range(16):
                            P.op("pe", lambda e, ng=ng, f=f: e.matmul(pbig[:, ng * 512:(ng + 1) * 512], lhsT=catT[:, f, :],
                                                                      rhs=wo[:, f, ng * 512:(ng + 1) * 512], start=(f == 0), stop=(f == 15)),
                                 reads=["c_catT", "c_wo"], writes=["c_pbig"])
                    P.op("dve", lambda e: e.tensor_tensor(out=h2[:], in0=pbig[:], in1=G1b[:], op=ALU.mult), reads=["c_pbig", "c_mod"], writes=["c_h2"])
                    P.op("pool", lambda e, xb=xb: e.tensor_tensor(out=xt[xb][:], in0=xt[xb][:], in1=h2[:], op=ALU.add), reads=[kx, "c_h2"], writes=[kx])
                    P.dma("sp", lambda e, i=i, xb=xb: e.dma_start(out=XN[i * 128:(i + 1) * 128, :], in_=xt[xb][:]), reads=[kx], writes=["*XN"])
                    rms_mod(xt[xb], kx, h2, "c_h2", A2b, S2b, "c_mod", tmp)
                    P.op("act", lambda e: e.copy(out=h2b[:], in_=h2[:]), reads=["c_h2"], writes=["c_h2b"])
                    P.dma("sp", lambda e, i=i: e.dma_start(out=H2[i * 128:(i + 1) * 128, :], in_=h2b[:]), reads=["c_h2b"], writes=["*H2"])
                    for kc in range(KC):
                        P.op("pe", lambda e, kc=kc: e.transpose(out=pbig[:, kc * 128:(kc + 1) * 128], in_=h2[:, kc * 128:(kc + 1) * 128], identity=ident[:]),
                             reads=["c_h2", "ident"], writes=["c_pbig"])
                    P.op("act", lambda e: e.copy(out=h2T[:].rearrange("p k t -> p (k t)"), in_=pbig[:]), reads=["c_pbig"], writes=["c_h2T"])
                    for kc in range(KC):
                        P.op("pe", lambda e, kc=kc: e.matmul(plog[:], lhsT=h2T[:, kc, :], rhs=wr[:, kc, :], start=(kc == 0), stop=(kc == KC - 1)),
                             reads=["c_h2T", "c_wr"], writes=["c_plog"])
                    P.op("dve", lambda e: e.tensor_tensor(out=lg[:], in0=plog[:], in1=brb[:], op=ALU.add), reads=["c_plog", "c_brb"], writes=["c_lg"])
                    P.op("dve", lambda e: e.tensor_reduce(out=sc1[:, 0:1], in_=lg[:], axis=AX.X, op=ALU.max, negate=True), reads=["c_lg"], writes=["c_sc1"])
                    P.op("act", lambda e: e.activation(out=ee[:], in_=lg[:], func=AF.Exp, bias=sc1[:, 0:1]), reads=["c_lg", "c_sc1"], writes=["c_ee"])
                    ev = ee[:].rearrange("p (g j) -> p g j", g=4)
                    e2v = e2[:].rearrange("p (g j) -> p g j", g=4)
                    P.op("dve", lambda e, ev=ev: e.tensor_reduce(out=m1[:], in_=ev, axis=AX.X, op=ALU.max), reads=["c_ee"], writes=["c_m1"])
                    P.op("dve", lambda e, ev=ev, e2v=e2v: e.tensor_tensor(out=e2v, in0=ev, in1=m1[:].unsqueeze(2).to_broadcast([128, 4, 4]), op=ALU.is_lt),
                         reads=["c_ee", "c_m1"], writes=["c_e2"])
                    P.op("dve", lambda e: e.tensor_tensor(out=e2[:], in0=e2[:], in1=ee[:], op=ALU.mult), reads=["c_e2", "c_ee"], writes=["c_e2"])
                    P.op("dve", lambda e, e2v=e2v: e.tensor_reduce(out=m2[:], in_=e2v, axis=AX.X, op=ALU.max), reads=["c_e2"], writes=["c_m2"])
                    P.op("dve", lambda e: e.tensor_tensor(out=gs[:], in0=m1[:], in1=m2[:], op=ALU.add), reads=["c_m1", "c_m2"], writes=["c_gs"])
                    P.op("dve", lambda e: e.tensor_reduce(out=sc1[:, 1:2], in_=gs[:], axis=AX.X, op=ALU.max), reads=["c_gs"], writes=["c_sc1"])
                    P.op("dve", lambda e: e.tensor_scalar(out=gs[:], in0=gs[:], scalar1=sc1[:, 1:2], scalar2=None, op0=ALU.is_equal),
                         reads=["c_gs", "c_sc1"], writes=["c_gs"])
                    P.op("dve", lambda e: e.reciprocal(out=sc1[:, 2:3], in_=sc1[:, 1:2]), reads=["c_sc1"], writes=["c_sc1"])
                    selv = sel_all[:, i, :].rearrange("p (g j) -> p g j", g=4)
                    P.op("dve", lambda e, ev=ev, selv=selv: e.tensor_tensor(out=selv, in0=ev, in1=m2[:].unsqueeze(2).to_broadcast([128, 4, 4]), op=ALU.is_ge),
                         reads=["c_ee", "c_m2"], writes=["c_sel"])
                    P.op("dve", lambda e, selv=selv: e.tensor_tensor(out=selv, in0=selv, in1=gs[:].unsqueeze(2).to_broadcast([128, 4, 4]), op=ALU.mult),
                         reads=["c_sel", "c_gs"], writes=["c_sel"])
                    P.op("dve", lambda e, i=i: e.scalar_tensor_tensor(out=wts_all[:, i, :], in0=ee[:], scalar=sc1[:, 2:3], in1=sel_all[:, i, :],
                                                                      op0=ALU.mult, op1=ALU.mult),
                         reads=["c_ee", "c_sc1", "c_sel"], writes=["c_wts"])

                dump("XN", XN)

            def body2(sb, ps):
                sel_all, wts_all = sel_top, wts_top
                fc = sb("c_fc", [128, NFC])
                P.dma("sp", lambda e: e.dma_start(out=fc[:], in_=fconst), writes=["c_fc"])
                tokid = fc[:, 0:NT]
                thr = fc[:, NT:NT + c.KB]
                bidx = fc[:, NT + c.KB:NT + c.KB + NB]
                tri = sb("c_tri", [128, 128])
                ones = sb("c_ones", [128, 128])
                P.op("pool", lambda e: e.memset(ones[:], 1.0), writes=["c_ones"])
                P.dma("sp", lambda e: e.dma_start(out=tri[:], in_=tri_d), writes=["c_tri"])
                NW = NTr * 16
                cum = sb("c_cum", [128, NT, 16])
                tot = sb("c_tot", [128, NT, 16])
                off = sb("c_off", [128, NT, 16])
                selw = sel_all[:, 0:NTr, :].rearrange("p t e -> p (t e)")
                pc2 = ps("c_pc2", [128, 1024])
                for (dst, lhs, kd) in ((cum, tri, "c_cum"), (tot, ones, "c_tot")):
                    for n0 in range(0, NW, 512):
                        n1 = min(NW, n0 + 512)
                        P.op("pe", lambda e, lhs=lhs, n0=n0, n1=n1: e.matmul(pc2[:, 0:n1 - n0], lhsT=lhs[:], rhs=selw[:, n0:n1], start=True, stop=True),
                             reads=["c_sel", "c_tri", "c_ones"], writes=["c_pc2"])
                        P.op("dve", lambda e, dst=dst, n0=n0, n1=n1: e.tensor_copy(out=dst[:, 0:NTr, :].rearrange("p t e -> p (t e)")[:, n0:n1], in_=pc2[:, 0:n1 - n0]),
                             reads=["c_pc2"], writes=[kd])
                P.op("dve", lambda e: e.memset(off[:, 0, :], 0.0), writes=["c_off"])
                for i in range(1, NTr):
                    P.op("dve", lambda e, i=i: e.tensor_tensor(out=off[:, i, :], in0=off[:, i - 1, :], in1=tot[:, i - 1, :], op=ALU.add),
                         reads=["c_off", "c_tot"], writes=["c_off"])
                ne = sb("c_ne", [128, 16])
                nbk = sb("c_nbk", [128, 16])
                bst = sb("c_bst", [128, 16])
                bend = sb("c_bend", [128, 16])
                cmpk = sb("c_cmpk", [128, 16, c.KB])
                P.op("dve", lambda e: e.tensor_tensor(out=ne[:], in0=off[:, NTr - 1, :], in1=tot[:, NTr - 1, :], op=ALU.add), reads=["c_off", "c_tot"], writes=["c_ne"])
                P.op("dve", lambda e: e.tensor_tensor(out=cmpk[:], in0=ne[:].unsqueeze(2).to_broadcast([128, 16, c.KB]),
                                                      in1=thr.unsqueeze(1).to_broadcast([128, 16, c.KB]), op=ALU.is_gt),
                     reads=["c_ne", "c_fc"], writes=["c_cmpk"])
                P.op("dve", lambda e: e.tensor_reduce(out=nbk[:], in_=cmpk[:], axis=AX.X, op=ALU.add), reads=["c_cmpk"], writes=["c_nbk"])
                P.op("dve", lambda e: e.memset(bst[:, 0:1], 0.0), writes=["c_bst"])
                for j in range(1, 16):
                    P.op("dve", lambda e, j=j: e.tensor_tensor(out=bst[:, j:j + 1], in0=bst[:, j - 1:j], in1=nbk[:, j - 1:j], op=ALU.add),
                         reads=["c_bst", "c_nbk"], writes=["c_bst"])
                P.op("dve", lambda e: e.tensor_tensor(out=bend[:], in0=bst[:], in1=nbk[:], op=ALU.add), reads=["c_bst", "c_nbk"], writes=["c_bend"])
                cmpb = sb("c_cmpb", [128, NB, 16])
                bef = sb("c_bef", [128, NB])
                bei = bei_top[li][:, 0:NB]
                state["bei"] = bei
                P.op("dve", lambda e: e.tensor_tensor(out=cmpb[:], in0=bend[:].unsqueeze(1).to_broadcast([128, NB, 16]),
                                                      in1=bidx.unsqueeze(2).to_broadcast([128, NB, 16]), op=ALU.is_le),
                     reads=["c_bend", "c_fc"], writes=["c_cmpb"])
                P.op("dve", lambda e: e.tensor_reduce(out=bef[:], in_=cmpb[:], axis=AX.X, op=ALU.add), reads=["c_cmpb"], writes=["c_bef"])
                P.op("dve", lambda e: e.tensor_scalar(out=bef[:], in0=bef[:], scalar1=float(c.NE - 1), scalar2=None, op0=ALU.min), reads=["c_bef"], writes=["c_bef"])
                P.op("dve", lambda e: e.tensor_copy(out=bei, in_=bef[:]), reads=["c_bef"], writes=["c_bei"])
                P.op("dve", lambda e: e.tensor_copy(out=bef_top[li][:, 0:NB], in_=bef[:]), reads=["c_bef"], writes=["bef_top%d" % li])
                pos = sb("c_pos", [128, NT, 16])
                P.op("dve", lambda e: e.tensor_scalar(out=bst[:], in0=bst[:], scalar1=float(c.BLK), scalar2=-1.0, op0=ALU.mult, op1=ALU.add),
                     reads=["c_bst"], writes=["c_bst"])
                P.op("dve", lambda e: e.tensor_tensor(out=pos[:, 0:NTr, :], in0=off[:, 0:NTr, :], in1=cum[:, 0:NTr, :], op=ALU.add),
                     reads=["c_off", "c_cum"], writes=["c_pos"])
                P.op("dve", lambda e: e.tensor_tensor(out=pos[:, 0:NTr, :], in0=pos[:, 0:NTr, :], in1=bst[:].unsqueeze(1).to_broadcast([128, NTr, 16]), op=ALU.add),
                     reads=["c_pos", "c_bst"], writes=["c_pos"])
                ksel = sb("c_ksel", [128, NT, 16])
                kv4 = ksel[:, 0:NTr, :].rearrange("p t (g j) -> p t g j", g=4)
                sv4 = sel_all[:, 0:NTr, :].rearrange("p t (g j) -> p t g j", g=4)
                P.op("dve", lambda e: e.memset(ksel[:], 0.0), writes=["c_ksel"])
                for j in range(1, 4):
                    P.op("dve", lambda e, j=j: e.tensor_tensor(out=kv4[:, :, :, j], in0=kv4[:, :, :, j - 1], in1=sv4[:, :, :, j - 1], op=ALU.add),
                         reads=["c_ksel", "c_sel"], writes=["c_ksel"])
                mk = [sb("c_mk%d" % k, [128, NT, 16]) for k in range(2)]
                P.op("dve", lambda e: e.tensor_tensor(out=mk[1][:, 0:NTr, :], in0=sel_all[:, 0:NTr, :], in1=ksel[:, 0:NTr, :], op=ALU.mult),
                     reads=["c_sel", "c_ksel"], writes=["c_mk1"])
                P.op("dve", lambda e: e.tensor_tensor(out=mk[0][:, 0:NTr, :], in0=sel_all[:, 0:NTr, :], in1=mk[1][:, 0:NTr, :], op=ALU.subtract),
                     reads=["c_sel", "c_mk1"], writes=["c_mk0"])
                tmpm = sb("c_tmpm", [128, NT, 16])
                posk = sb("c_posk", [128, NT, 2])
                gatek = sb("c_gatek", [128, NT, 2])
                posi = sb("c_posi", [128, NT, 2], I32)
                rec = sb("c_rec", [128, NT, 2, 4], I32)
                dstf = sb("c_dstf", [128, NT, 2])
                P.op("pool", lambda e: e.memset(rec[:], 0), writes=["c_rec"])
                for k in range(2):
                    P.op("dve", lambda e, k=k: e.tensor_tensor(out=tmpm[:, 0:NTr, :], in0=mk[k][:, 0:NTr, :], in1=pos[:, 0:NTr, :], op=ALU.mult),
                         reads=["c_mk%d" % k, "c_pos"], writes=["c_tmpm"])
                    P.op("dve", lambda e, k=k: e.tensor_reduce(out=posk[:, 0:NTr, k], in_=tmpm[:, 0:NTr, :], axis=AX.X, op=ALU.add),
                         reads=["c_tmpm"], writes=["c_posk"])
                    P.op("dve", lambda e, k=k: e.tensor_tensor(out=tmpm[:, 0:NTr, :], in0=mk[k][:, 0:NTr, :], in1=wts_all[:, 0:NTr, :], op=ALU.mult),
                         reads=["c_mk%d" % k, "c_wts"], writes=["c_tmpm"])
                    P.op("dve", lambda e, k=k: e.tensor_reduce(out=gatek[:, 0:NTr, k], in_=tmpm[:, 0:NTr, :], axis=AX.X, op=ALU.add),
                         reads=["c_tmpm"], writes=["c_gatek"])
                    P.op("dve", lambda e, k=k: e.tensor_scalar(out=dstf[:, 0:NTr, k], in0=tokid[:, 0:NTr], scalar1=float(k * TC), scalar2=None, op0=ALU.add),
                         reads=["c_fc"], writes=["c_dstf"])
                    P.op("dve", lambda e, k=k: e.tensor_copy(out=rec[:, 0:NTr, k, 0], in_=tokid[:, 0:NTr]), reads=["c_fc", "c_rec"], writes=["c_rec"])
                    P.op("dve", lambda e, k=k: e.tensor_copy(out=rec[:, 0:NTr, k, 1], in_=dstf[:, 0:NTr, k]), reads=["c_dstf", "c_rec"], writes=["c_rec"])
                    P.op("dve", lambda e, k=k: e.tensor_copy(out=rec[:, 0:NTr, k, 2:3].bitcast(F32).rearrange("p t o -> p (t o)"), in_=gatek[:, 0:NTr, k]),
                         reads=["c_gatek", "c_rec"], writes=["c_rec"])
                P.op("dve", lambda e: e.tensor_copy(out=posi[:, 0:NTr, :], in_=posk[:, 0:NTr, :]), reads=["c_posk"], writes=["c_posi"])
                P.dma("sp", lambda e: e.dma_start(out=slots[0:NB * c.BLK, :], in_=slots_init[0:NB * c.BLK, :]), writes=["*slots"])
                P.dma("sp", lambda e: e.dma_start(out=H2[TC:TC + 1, :], in_=zrow), writes=["*H2"])
                for i in range(NTr):
                    for k in range(2):
                        P.dma("pool", lambda e, i=i, k=k: e.indirect_dma_start(
                            out=slots, out_offset=bass.IndirectOffsetOnAxis(ap=posi[:, i, k:k + 1], axis=0), in_=rec[:, i, k, :], in_offset=None),
                            reads=["c_posi", "c_rec", "*slots"], writes=["*sl%d_%d" % (i, k)])
                state["slkeys"] = ["*sl%d_%d" % (i, k) for i in range(NTr) for k in range(2)]
                dump("slots", slots)
                dump("H2", H2)

            def body3(sb, ps):
                bei = bei_top[li]
                rec = [sb("e_rec%d" % i, [128, 4, 4], I32) for i in range(2)]
                xg = [sb("e_xg%d" % i, [128, D], BF16) for i in range(2)]
                xT = [sb("e_xT%d" % i, [128, 16, 512], BF16) for i in range(2)]
                wgs = [sb("e_wgs%d" % i, [128, 16, 128]) for i in range(2)]
                wus = [sb("e_wus%d" % i, [128, 16, 128]) for i in range(2)]
                wgb = [sb("e_wgb%d" % i, [128, 16, 128], BF16) for i in range(2)]
                wub = [sb("e_wub%d" % i, [128, 16, 128], BF16) for i in range(2)]
                wds = [sb("e_wds%d" % i, [128, D]) for i in range(2)]
                wdb = sb("e_wdb", [128, c.JC, D], BF16)
                sg = sb("e_sg", [128, 512])
                hT = sb("e_hT", [128, c.JC, 512], BF16)
                ys = [sb("e_ys%d" % i, [128, D]) for i in range(2)]
                pxt = ps("e_pxt", [128, 16, 128], BF16)
                pa = [ps("e_pa%d" % i, [128, 512]) for i in range(2)]
                pb = [ps("e_pb%d" % i, [128, 512]) for i in range(2)]
                py = ps("e_py", [128, 1024])
                wg_rows = w_gate_g[li].rearrange("r (j n) -> (r j) n", n=128)
                wu_rows = w_up_g[li].rearrange("r (j n) -> (r j) n", n=128)
                o_ = NT + c.KB + NBM
                sidx_g = fc_top[:, o_:o_ + 176]
                sidx_d = fc_top[:, o_ + 176:o_ + 187]
                ebg = sb("e_ebg", [128, NB])
                ebd = sb("e_ebd", [128, NB])
                ixf = [sb("e_ixf%d" % i, [128, 187]) for i in range(2)]
                ixi = [sb("e_ixi%d" % i, [128, 187], I32) for i in range(2)]
                P.op("dve", lambda e: e.tensor_scalar(out=ebg[:], in0=bef_top[li][:, 0:NB], scalar1=float(D * 11), scalar2=None, op0=ALU.mult),
                     reads=["bef_top%d" % li], writes=["e_ebg"])
                P.op("dve", lambda e: e.tensor_scalar(out=ebd[:], in0=bef_top[li][:, 0:NB], scalar1=float(c.DE), scalar2=None, op0=ALU.mult),
                     reads=["bef_top%d" % li], writes=["e_ebd"])
                it = 0
                for b in range(NB):
                    rb = b % 2
                    P.dma("sp", lambda e, b=b, rb=rb: e.dma_start(out=rec[rb][:], in_=slots[b * c.BLK:(b + 1) * c.BLK, :].rearrange("(s p) f -> p s f", p=128)),
                          reads=["*slots"] + state["slkeys"], writes=["e_rec%d" % rb])
                    for s in range(4):
                        gb = (b * 4 + s) % 2
                        P.dma("pool", lambda e, s=s, rb=rb, gb=gb: e.indirect_dma_start(
                            out=xg[gb][:], out_offset=None, in_=H2, in_offset=bass.IndirectOffsetOnAxis(ap=rec[rb][:, s, 0:1], axis=0)),
                            reads=["e_rec%d" % rb, "*H2"], writes=["e_xg%d" % gb])
                        for kc in range(KC):
                            P.op("pe", lambda e, kc=kc, gb=gb: e.transpose(out=pxt[:, kc, :], in_=xg[gb][:, kc * 128:(kc + 1) * 128], identity=identb[:]),
                                 reads=["e_xg%d" % gb, "identb"], writes=["e_pxt"])
                        P.op("act" if s % 2 == 0 else "dve", lambda e, s=s, rb=rb: (e.copy if hasattr(e, "copy") else e.tensor_copy)(
                            out=xT[rb][:, :, s * 128:(s + 1) * 128], in_=pxt[:]), reads=["e_pxt"], writes=["e_xT%d" % rb])
                    P.op("dve", lambda e, b=b, rb=rb: e.tensor_scalar(out=ixf[rb][:, 0:176], in0=sidx_g, scalar1=ebg[:, b:b + 1], scalar2=None, op0=ALU.add),
                         reads=["e_ebg", "fc_top"], writes=["e_ixf%d" % rb])
                    P.op("dve", lambda e, b=b, rb=rb: e.tensor_scalar(out=ixf[rb][:, 176:187], in0=sidx_d, scalar1=ebd[:, b:b + 1], scalar2=None, op0=ALU.add),
                         reads=["e_ebd", "fc_top"], writes=["e_ixf%d" % rb])
                    P.op("dve", lambda e, rb=rb: e.tensor_copy(out=ixi[rb][:], in_=ixf[rb][:]), reads=["e_ixf%d" % rb], writes=["e_ixi%d" % rb])

                    def issue_loads(j, wb_, b=b, rb=rb):
                        for kc in range(KC):
                            for (dst, rows_v, kname) in ((wgs, wg_rows, "e_wgs"), (wus, wu_rows, "e_wus")):
                                P.dma("pool", lambda e, dst=dst, rows_v=rows_v, kc=kc, j=j, wb_=wb_, rb=rb: e.indirect_dma_start(
                                    out=dst[wb_][:, kc, :], out_offset=None, in_=rows_v,
                                    in_offset=bass.IndirectOffsetOnAxis(ap=ixi[rb][:, kc * 11 + j:kc * 11 + j + 1], axis=0)),
                                    reads=["e_ixi%d" % rb], writes=["%s%d_%d" % (kname, wb_, kc)])
                        P.dma("pool", lambda e, j=j, wb_=wb_, rb=rb: e.indirect_dma_start(
                            out=wds[wb_][:], out_offset=None, in_=w_down_g[li],
                            in_offset=bass.IndirectOffsetOnAxis(ap=ixi[rb][:, 176 + j:176 + j + 1], axis=0)),
                            reads=["e_ixi%d" % rb], writes=["e_wds%d" % wb_])
                    issue_loads(0, it % 2)
                    for j in range(c.JC):
                        wb_ = it % 2
                        it += 1
                        if j + 1 < c.JC:
                            issue_loads(j + 1, it % 2)
                        P.op("act", lambda e, wb_=wb_: e.copy(out=wgb[wb_][:], in_=wgs[wb_][:]), reads=["e_wgs%d_%d" % (wb_, kc) for kc in range(KC)], writes=["e_wgb%d" % wb_])
                        P.op("dve", lambda e, wb_=wb_: e.tensor_copy(out=wub[wb_][:], in_=wus[wb_][:]), reads=["e_wus%d_%d" % (wb_, kc) for kc in range(KC)], writes=["e_wub%d" % wb_])
                        P.op("act", lambda e, wb_=wb_, j=j: e.copy(out=wdb[:, j, :], in_=wds[wb_][:]), reads=["e_wds%d" % wb_], writes=["e_wdb%d" % j])
                        for kc in range(KC):
                            P.op("pe", lambda e, kc=kc, wb_=wb_, rb=rb: e.matmul(pa[wb_][:], lhsT=wgb[wb_][:, kc, :], rhs=xT[rb][:, kc, :],
                                                                                 start=(kc == 0), stop=(kc == KC - 1)),
                                 reads=["e_wgb%d" % wb_, "e_xT%d" % rb], writes=["e_pa%d" % wb_])
                        for kc in range(KC):
                            P.op("pe", lambda e, kc=kc, wb_=wb_, rb=rb: e.matmul(pb[wb_][:], lhsT=wub[wb_][:, kc, :], rhs=xT[rb][:, kc, :],
                                                                                 start=(kc == 0), stop=(kc == KC - 1)),
                                 reads=["e_wub%d" % wb_, "e_xT%d" % rb], writes=["e_pb%d" % wb_])
                        P.op("act", lambda e, wb_=wb_: e.activation(out=sg[:], in_=pa[wb_][:], func=AF.Silu), reads=["e_pa%d" % wb_], writes=["e_sg"])
                        P.op("dve", lambda e, wb_=wb_, j=j: e.tensor_tensor(out=hT[:, j, :], in0=sg[:], in1=pb[wb_][:], op=ALU.mult),
                             reads=["e_sg", "e_pb%d" % wb_], writes=["e_hT"])
                    for s in range(4):
                        yb = (b * 4 + s) % 2
                        for nh in range(2):
                            for ng in range(2):
                                for j in range(c.JC):
                                    P.op("pe", lambda e, s=s, nh=nh, ng=ng, j=j: e.matmul(
                                        py[:, ng * 512:(ng + 1) * 512], lhsT=hT[:, j, s * 128:(s + 1) * 128],
                                        rhs=wdb[:, j, nh * 1024 + ng * 512:nh * 1024 + (ng + 1) * 512], start=(j == 0), stop=(j == c.JC - 1)),
                                        reads=["e_hT", "e_wdb%d" % j], writes=["e_py"])
                            P.op("dve", lambda e, s=s, nh=nh, yb=yb, rb=rb: e.tensor_scalar(
                                out=ys[yb][:, nh * 1024:(nh + 1) * 1024], in0=py[:], scalar1=rec[rb][:, s, 2:3].bitcast(F32), scalar2=None, op0=ALU.mult),
                                reads=["e_py", "e_rec%d" % rb], writes=["e_ys%d" % yb])
                        P.dma("pool", lambda e, s=s, yb=yb, rb=rb: e.indirect_dma_start(
                            out=Y, out_offset=bass.IndirectOffsetOnAxis(ap=rec[rb][:, s, 1:2], axis=0), in_=ys[yb][:], in_offset=None),
                            reads=["e_ys%d" % yb, "e_rec%d" % rb], writes=["*Y"])
                dump("Y", Y)

            def body4(sb, ps):
                G2b = sb("f_G2", [128, D])
                xn = [sb("f_xn%d" % i, [128, D]) for i in range(2)]
                y0 = [sb("f_y0%d" % i, [128, D]) for i in range(2)]
                y1 = [sb("f_y1%d" % i, [128, D]) for i in range(2)]
                cur_mod = [None]
                for i in range(NTr):
                    isctx = i >= NTL
                    src = 1 if isctx else 0
                    if cur_mod[0] != src:
                        cur_mod[0] = src
                        load_bvec(G2b[:], modvec[li, src, 5:6, :], "f_mod")
                    b = i % 2
                    P.dma("sp", lambda e, i=i, b=b: e.dma_start(out=xn[b][:], in_=XN[i * 128:(i + 1) * 128, :]), reads=["*XN"], writes=["f_xn%d" % b])
                    P.dma("sp", lambda e, i=i, b=b: e.dma_start(out=y0[b][:], in_=Y[i * 128:(i + 1) * 128, :]), reads=["*Y"], writes=["f_y0%d" % b])
                    P.dma("sp", lambda e, i=i, b=b: e.dma_start(out=y1[b][:], in_=Y[TC + i * 128:TC + (i + 1) * 128, :]), reads=["*Y"], writes=["f_y1%d" % b])
                    P.op("pool", lambda e, b=b: e.tensor_tensor(out=y0[b][:], in0=y0[b][:], in1=y1[b][:], op=ALU.add),
                         reads=["f_y0%d" % b, "f_y1%d" % b], writes=["f_y0%d" % b])
                    P.op("dve", lambda e, b=b: e.tensor_tensor(out=y0[b][:], in0=y0[b][:], in1=G2b[:], op=ALU.mult),
                         reads=["f_y0%d" % b, "f_mod"], writes=["f_y0%d" % b])
                    P.op("dve", lambda e, b=b: e.tensor_tensor(out=xn[b][:], in0=xn[b][:], in1=y0[b][:], op=ALU.add),
                         reads=["f_y0%d" % b, "f_xn%d" % b], writes=["f_xn%d" % b])
                    if li == 0:
                        dst = X1[i * 128:(i + 1) * 128, :]
                        wk = "*X1"
                    else:
                        dst = y_out[i * 128:(i + 1) * 128, :]
                        wk = "*y_out"
                    P.dma("sp", lambda e, dst=dst, b=b: e.dma_start(out=dst, in_=xn[b][:]), reads=["f_xn%d" % b], writes=[wk])
                if li == 0:
                    dump("X1", X1)

            bei_d = dscr("bei_d%d" % li, [128, c.nblk_max()], I32)
            state["bei_d"] = bei_d

            def body2w(sb, ps):
                body2(sb, ps)
                P.dma("sp", lambda e: e.dma_start(out=bei_d[:, 0:NB], in_=state["bei"]), reads=["c_bei"], writes=["*bei"])
            phase(body1)
            phase(body2w)
            if li == 0:
                dump("bei", bei_d)
            if stop_after == ("C1", li):
                return True
            phase(body3)
            phase(body4)
            return False

        phase(phase_M)
        done = False
        for li in range(2):
            if done:
                break
            phase_A(li, 0)
            phase_A(li, 1)
            phase_G(li)
            if stop_after == ("G", li):
                break
            phase_B(li)
            if stop_after == ("B", li):
                break
            done = phase_C(li)
        P.barrier()
        P.flush()
        print("[kernel] ops=%d waits=%d" % (P.n_ops, P.n_wait))
    return nc


def _pool_mats(n_seq, T0):
    wins = (2, 4, 8, 16)
    main = np.zeros((4, 128, 128), np.float32)
    halo = np.zeros((4, 32, 128), np.float32)
    for g, w in enumerate(wins):
        half = w // 2
        for t in range(128):
            pos = T0 + t
            lo = min(max(pos - half, 0), n_seq - 1)
            hi = min(max(pos - half + w - 1, 0), n_seq - 1)
            wgt = np.float32(1.0) / np.float32(hi - lo + 1)
            for p2 in range(lo, hi + 1):
                if T0 <= p2 < T0 + 128:
                    main[g, p2 - T0, t] += wgt
                elif p2 < T0:
                    halo[g, p2 - (T0 - 16), t] += wgt
                else:
                    halo[g, 16 + p2 - (T0 + 128), t] += wgt
            main[g, t, t] -= 1.0
    return main, halo


def _host_inputs(cfg, inputs, with_moe=True):
    c = cfg
    NL, CL, TC, NT, NTL = c.NL, c.CL, c.TC, c.NT, c.NTL
    f = lambda a: np.ascontiguousarray(np.asarray(a), dtype=np.float32)
    x = f(inputs["x"])
    ctx = f(inputs["ctx"])
    cc = f(inputs["c"])
    c_ctx = f(inputs["c_ctx"])
    SEQ = x.shape[1]
    assert SEQ == 4 * NL
    shared = {
        "q_norm": f(inputs["q_norm"]), "k_norm": f(inputs["k_norm"]),
        "lam_qk": f(inputs["lam_qk"]).reshape(2, 256), "sub_norm": f(inputs["sub_norm"]), "w_pool": f(inputs["w_pool"]),
        "pool_scale": np.ascontiguousarray(f(inputs["pool_scale"]).reshape(2, 8, 128).transpose(0, 2, 1)), "w_router": f(inputs["w_router"]),
        "b_router": f(inputs["b_router"]).reshape(1, 16),
        "ident": np.eye(128, dtype=np.float32),
        "tri": np.triu(np.ones((128, 128), np.float32)),
        "zrow": np.zeros((1, c.D), ml_dtypes.bfloat16),
            }
    w_mod = f(inputs["w_mod"]).reshape(2, c.D, 6, 4, 512)
    b_mod = f(inputs["b_mod"]).reshape(2, 6, 4, 512)
    norm1 = f(inputs["norm1"]).reshape(2, 4, 512)
    norm2 = f(inputs["norm2"]).reshape(2, 4, 512)
    for l_ in range(2):
        shared["w_in%d" % l_] = f(inputs["w_in"][l_])
        shared["w_out%d" % l_] = f(inputs["w_out"][l_])
        if with_moe:
            shared["w_gate%d" % l_] = f(inputs["w_gate"][l_]).reshape(c.NE * c.D, c.DE)
            shared["w_up%d" % l_] = f(inputs["w_up"][l_]).reshape(c.NE * c.D, c.DE)
            shared["w_down%d" % l_] = f(inputs["w_down"][l_]).reshape(c.NE * c.DE, c.D)
    NBM = c.nblk_max()
    fconst = np.zeros((128, NT + c.KB + NBM + 176 + 11), np.float32)
    fconst[:, 0:NT] = np.arange(NT)[None, :] * 128 + np.arange(128)[:, None]
    fconst[:, NT:NT + c.KB] = (np.arange(c.KB) * c.BLK)[None, :]
    fconst[:, NT + c.KB:NT + c.KB + NBM] = np.arange(NBM)[None, :]
    o_ = NT + c.KB + NBM
    pp_ = np.arange(128)[:, None, None]
    fconst[:, o_:o_ + 176] = ((np.arange(16)[None, :, None] * 128 + pp_) * 11 + np.arange(11)[None, None, :]).reshape(128, 176)
    fconst[:, o_ + 176:o_ + 187] = np.arange(11)[None, :] * 128 + np.arange(128)[:, None]
    shared["fconst"] = fconst
    si = np.zeros((NBM * c.BLK, 4), np.int32)
    si[:, 0] = TC
    si[:, 1] = c.DUMP0 + (np.arange(NBM * c.BLK) % 128)
    shared["slots_init"] = si
    nf = 16
    inv = (np.float32(10000.0) ** (-(np.arange(nf, dtype=np.float32) / np.float32(nf)))).astype(np.float32)
    m_int, h_int = _pool_mats(1 << 20, 1 << 10)
    m_c0, h_c0 = _pool_mats(CL, 0)
    m_c1, h_c1 = _pool_mats(CL, 128)
    in_maps = []
    for core in range(8):
        b, r = core // 4, core % 4
        d = dict(shared)
        d["x_in"] = x[b, r * NL:(r + 1) * NL]
        d["ctx_in"] = ctx[b]
        d["cvec"] = np.stack([cc[b], c_ctx]).astype(np.float32)
        d["w_mod_s"] = np.ascontiguousarray(w_mod[:, :, :, r, :])
        d["b_mod_s"] = np.ascontiguousarray(b_mod[:, :, r, :])
        mm = np.ones((2, 6, 512), np.float32)
        ma = np.zeros((2, 6, 512), np.float32)
        mm[:, 1] = norm1[:, r]
        mm[:, 4] = norm2[:, r]
        ma[:, 1] = 1.0
        ma[:, 4] = 1.0
        d["mod_mul_s"] = mm
        d["mod_add_s"] = ma
        mi = np.zeros((128, 1), np.int32)
        n_ = 0
        for l_ in range(2):
            for s_ in range(2):
                for w_ in range(6):
                    for r_ in range(4):
                        mi[n_, 0] = ((r_ * 2 + l_) * 2 + s_) * 6 + w_
                        n_ += 1
        d["mod_idx"] = mi
        pos = (r * NL + np.arange(NL)).astype(np.int64)
        row = (pos // 64).astype(np.float32)
        col = (pos % 64).astype(np.float32)
        cs = np.zeros((TC, 64), np.float32)
        cs[:NL, 0:16] = np.cos(row[:, None] * inv[None, :])
        cs[:NL, 16:32] = np.cos(col[:, None] * inv[None, :])
        cs[:NL, 32:48] = np.sin(row[:, None] * inv[None, :])
        cs[:NL, 48:64] = np.sin(col[:, None] * inv[None, :])
        cs[NL:, 0:32] = 1.0
        d["cs_tab"] = cs
        m0, h0 = _pool_mats(SEQ, r * NL)
        m2, h2 = _pool_mats(SEQ, (r + 1) * NL - 128)
        d["pm_main"] = np.stack([m0, m_int, m2, m_c0, m_c1])
        d["pm_halo"] = np.stack([h0, h_int, h2, h_c0, h_c1])
        ui = np.zeros((128, 1), np.int32)
        for j in range(16):
            ui[j, 0] = (r - 1) * 32 + 16 + j if r > 0 else 0
            ui[16 + j, 0] = (r + 1) * 32 + j if r < 3 else 0
        d["uedge_idx"] = ui
        in_maps.append(d)
    return in_maps


_NC_CACHE = {}


def kernel(**inputs):
    x = np.asarray(inputs["x"])
    B, SEQ, D = x.shape
    cfg = Cfg(NL=SEQ // 4)
    key = cfg.NL
    if key not in _NC_CACHE:
        _NC_CACHE[key] = build_program(cfg)
    nc = _NC_CACHE[key]
    in_maps = _host_inputs(cfg, inputs)
    res = run_bass_kernel_spmd(nc, in_maps, core_ids=list(range(8)))
    out = np.empty((B, SEQ, D), np.float32)
    for core in range(8):
        b, r = core // 4, core % 4
        out[b, r * cfg.NL:(r + 1) * cfg.NL] = res.results[core]["y_out"]
    return out
```
